# Optimizing a Trainium2 kernel written in Bass

```python
import math
import jax, jax.numpy as jnp
from jax import lax
import numpy as np

D_MODEL = 1024
BATCH = 2
SEQ = 16384
DEPTH = 2

GRID_W = 64
CTX_LEN = 256
RMS_EPS = 1e-6
SCAN_CHUNK = 128
S5_WIDTH = 512
S5_GROUP = 16
S5_GROUPS = S5_WIDTH // S5_GROUP
S5_STATE = 64
RET_HEADS = 4
RET_HEAD_DIM = 128
RET_WIDTH = RET_HEADS * RET_HEAD_DIM
EVEN_IN = S5_WIDTH + 4 * RET_WIDTH
EVEN_MIX = S5_WIDTH + RET_WIDTH
ROPE_BASE = 10000.0
NA_HEADS = 16
NA_HEAD_DIM = 64
NA_WIDTH = NA_HEADS * NA_HEAD_DIM
NA_WIN_ROWS = 8
NA_WIN_COLS = 16
N_EXPERTS = 32
MOE_TOP_K = 4
MOE_FF = D_MODEL
SWIGLU_LIMIT = 7.0
SWIGLU_ALPHA = 1.702
MOE_BLOCK = 128

kernel_name = "hybrid_s5_retention_natten_moe_block"

F32 = jnp.float32


def rms_norm(x, w):
    xf = x.astype(F32)
    y = xf * lax.rsqrt(jnp.mean(xf * xf, axis=-1, keepdims=True) + RMS_EPS)
    return (y * w.astype(F32)).astype(x.dtype)


def modulate(h, shift, scale):
    return h * (1 + scale) + shift


def rope_1d(x, pos):
    half = x.shape[-1] // 2
    freq = ROPE_BASE ** (-jnp.arange(half, dtype=F32) / half)
    ang = pos[:, None] * freq[None, :]
    cos, sin = jnp.cos(ang), jnp.sin(ang)
    xf = x.astype(F32)
    x1, x2 = xf[..., :half], xf[..., half:]
    return jnp.concatenate([x1 * cos - x2 * sin, x1 * sin + x2 * cos], axis=-1).astype(x.dtype)


def axial_rope_2d(x):
    t = jnp.arange(x.shape[2])
    row = (t // GRID_W).astype(F32)
    col = (t % GRID_W).astype(F32)
    h = x.shape[-1] // 2
    return jnp.concatenate([rope_1d(x[..., :h], row), rope_1d(x[..., h:], col)], axis=-1)


def s5_discretize(lam_re, lam_im, log_step, b_re, b_im):
    lam_re = jnp.minimum(lam_re.astype(F32), -1e-4)
    lam_im = lam_im.astype(F32)
    step = jnp.exp(log_step.astype(F32))[:, None]
    mag = jnp.exp(lam_re * step)
    ang = lam_im * step
    a_re, a_im = mag * jnp.cos(ang), mag * jnp.sin(ang)
    den = lam_re * lam_re + lam_im * lam_im
    n_re, n_im = a_re - 1.0, a_im
    co_re = (n_re * lam_re + n_im * lam_im) / den
    co_im = (n_im * lam_re - n_re * lam_im) / den
    b_re, b_im = b_re.astype(F32), b_im.astype(F32)
    bb_re = co_re[..., None] * b_re - co_im[..., None] * b_im
    bb_im = co_re[..., None] * b_im + co_im[..., None] * b_re
    return a_re, a_im, bb_re, bb_im


def complex_linear_combine(e1, e2):
    a1r, a1i, b1r, b1i = e1
    a2r, a2i, b2r, b2i = e2
    return (a2r * a1r - a2i * a1i, a2r * a1i + a2i * a1r,
            a2r * b1r - a2i * b1i + b2r, a2r * b1i + a2i * b1r + b2i)


def s5_scan(u, a_re, a_im, bb_re, bb_im, c_re, c_im, h_re, h_im):
    bsz, seq_len, g, i = u.shape
    n = seq_len // SCAN_CHUNK
    uc = u.reshape(bsz, n, SCAN_CHUNK, g, i).transpose(1, 0, 2, 3, 4)

    def step(carry, u_blk):
        hr, hi = carry
        bu_re = jnp.einsum('bcgi,gpi->bcgp', u_blk, bb_re)
        bu_im = jnp.einsum('bcgi,gpi->bcgp', u_blk, bb_im)
        bu_re = bu_re.at[:, 0].add(a_re * hr - a_im * hi)
        bu_im = bu_im.at[:, 0].add(a_re * hi + a_im * hr)
        ar = jnp.broadcast_to(a_re, bu_re.shape)
        ai = jnp.broadcast_to(a_im, bu_im.shape)
        _, _, sr, si = lax.associative_scan(complex_linear_combine, (ar, ai, bu_re, bu_im), axis=1)
        y = jnp.einsum('bcgp,gip->bcgi', sr, c_re) - jnp.einsum('bcgp,gip->bcgi', si, c_im)
        return (sr[:, -1], si[:, -1]), y

    (hr, hi), y = lax.scan(step, (h_re, h_im), uc)
    return y.transpose(1, 0, 2, 3, 4).reshape(bsz, seq_len, g, i), hr, hi


def s5_mixer(u_lat, u_ctx, lam_re, lam_im, log_step, b_re, b_im, c_re, c_im, d_skip, w_glu):
    def grouped(u):
        return u.astype(F32).reshape(u.shape[0], u.shape[1], S5_GROUPS, S5_GROUP)
    ul, uc = grouped(u_lat), grouped(u_ctx)
    zeros = jnp.zeros((ul.shape[0], S5_GROUPS, S5_STATE), F32)
    d = d_skip.astype(F32).reshape(S5_GROUPS, S5_GROUP)
    y_lat, y_ctx = ul * d, uc * d
    for dr in range(2):
        a_re, a_im, bb_re, bb_im = s5_discretize(lam_re[dr], lam_im[dr], log_step[dr], b_re[dr], b_im[dr])
        cr, ci = c_re[dr].astype(F32), c_im[dr].astype(F32)
        flip = (lambda t: jnp.flip(t, axis=1)) if dr == 1 else (lambda t: t)
        yc, hr, hi = s5_scan(flip(uc), a_re, a_im, bb_re, bb_im, cr, ci, zeros, zeros)
        yl, _, _ = s5_scan(flip(ul), a_re, a_im, bb_re, bb_im, cr, ci, hr, hi)
        y_ctx = y_ctx + flip(yc)
        y_lat = y_lat + flip(yl)

    def glu(y, like):
        y = jax.nn.gelu(y.reshape(y.shape[0], y.shape[1], S5_WIDTH))
        return (y * jax.nn.sigmoid(y @ w_glu.astype(F32))).astype(like.dtype)
    return glu(y_lat, u_lat), glu(y_ctx, u_ctx)


def retention_scan(q, k, v, log_g, s0):
    bsz, h, seq_len, _ = q.shape
    dv = v.shape[-1]
    c = SCAN_CHUNK
    n = seq_len // c
    idx = jnp.arange(c, dtype=F32)
    diff = idx[:, None] - idx[None, :]
    inner = jnp.where(diff >= 0, jnp.exp(log_g[:, None, None] * jnp.maximum(diff, 0.0)), 0.0)
    q_dec = jnp.exp(log_g[:, None] * (idx + 1.0))[..., None]
    k_dec = jnp.exp(log_g[:, None] * (c - 1.0 - idx))[..., None]
    blk_dec = jnp.exp(log_g * c)[:, None, None]

    def chunks(t):
        return t.reshape(bsz, h, n, c, t.shape[-1]).transpose(2, 0, 1, 3, 4)

    def step(s, blk):
        qb, kb, vb = blk
        att = jnp.einsum('bhid,bhjd->bhij', qb, kb) * inner
        o = jnp.einsum('bhij,bhje->bhie', att, vb) + jnp.einsum('bhid,bhde->bhie', qb, s) * q_dec
        s = blk_dec * s + jnp.einsum('bhjd,bhje->bhde', kb * k_dec, vb)
        return s, o

    s, o = lax.scan(step, s0, (chunks(q), chunks(k), chunks(v)))
    return o.transpose(1, 2, 0, 3, 4).reshape(bsz, h, seq_len, dv), s


def retention_mixer(q_lat, k_lat, v_lat, g_lat, q_ctx, k_ctx, v_ctx, g_ctx, log_decay):
    def heads(t):
        return t.reshape(t.shape[0], t.shape[1], RET_HEADS, RET_HEAD_DIM).transpose(0, 2, 1, 3).astype(F32)
    scale = RET_HEAD_DIM ** -0.5
    ql = axial_rope_2d(heads(q_lat))
    kl = axial_rope_2d(heads(k_lat)) * scale
    qc, kc = heads(q_ctx), heads(k_ctx) * scale
    vl, vc = heads(v_lat), heads(v_ctx)
    s0 = jnp.zeros((ql.shape[0], RET_HEADS, RET_HEAD_DIM, RET_HEAD_DIM), F32)
    o_lat = jnp.zeros_like(vl)
    o_ctx = jnp.zeros_like(vc)
    for dr in range(2):
        lg = log_decay[dr].astype(F32)
        flip = (lambda t: jnp.flip(t, axis=2)) if dr == 1 else (lambda t: t)
        oc, sc = retention_scan(flip(qc), flip(kc), flip(vc), lg, s0)
        ol, _ = retention_scan(flip(ql), flip(kl), flip(vl), lg, sc)
        o_ctx = o_ctx + flip(oc)
        o_lat = o_lat + flip(ol)

    def finish(o, g):
        o = o.transpose(0, 2, 1, 3)
        o = o * lax.rsqrt(jnp.mean(o * o, axis=-1, keepdims=True) + RMS_EPS)
        o = o.reshape(o.shape[0], o.shape[1], RET_WIDTH)
        return (o * jax.nn.silu(g.astype(F32))).astype(g.dtype)
    return finish(o_lat, g_lat), finish(o_ctx, g_ctx)


def even_mixer(h_lat, h_ctx, w_in, w_out, lam_re, lam_im, log_step, b_re, b_im, c_re, c_im, d_skip, w_glu, log_decay):
    def parts(p):
        u = p[..., :S5_WIDTH]
        q, k, v, g = jnp.split(p[..., S5_WIDTH:], 4, axis=-1)
        return u, q, k, v, g
    ul, ql, kl, vl, gl = parts(h_lat @ w_in)
    uc, qc, kc, vc, gc = parts(h_ctx @ w_in)
    a_lat, a_ctx = s5_mixer(ul, uc, lam_re, lam_im, log_step, b_re, b_im, c_re, c_im, d_skip, w_glu)
    r_lat, r_ctx = retention_mixer(ql, kl, vl, gl, qc, kc, vc, gc, log_decay)
    o_lat = jnp.concatenate([a_lat, r_lat], axis=-1) @ w_out
    o_ctx = jnp.concatenate([a_ctx, r_ctx], axis=-1) @ w_out
    return o_lat, o_ctx


def na_mixer(h_lat, h_ctx, w_qkv, w_o, rpb, need_ctx_out):
    bsz, seq_len, _ = h_lat.shape
    rows = seq_len // GRID_W
    kr, kc = min(NA_WIN_ROWS, rows), NA_WIN_COLS
    scale = NA_HEAD_DIM ** -0.5
    qkv = (h_lat @ w_qkv).reshape(bsz, rows, GRID_W, 3, NA_HEADS, NA_HEAD_DIM)
    qg = qkv[:, :, :, 0].transpose(0, 3, 1, 2, 4) * scale
    kg = qkv[:, :, :, 1].transpose(0, 3, 1, 2, 4)
    vg = qkv[:, :, :, 2].transpose(0, 3, 1, 2, 4)
    n_ctx = h_ctx.shape[1]
    cqkv = (h_ctx @ w_qkv).reshape(bsz, n_ctx, 3, NA_HEADS, NA_HEAD_DIM)
    q_ctx = cqkv[:, :, 0].transpose(0, 2, 1, 3) * scale
    k_ctx = cqkv[:, :, 1].transpose(0, 2, 1, 3)
    v_ctx = cqkv[:, :, 2].transpose(0, 2, 1, 3)

    col = jnp.arange(GRID_W)
    col_start = jnp.clip(col - kc // 2, 0, GRID_W - kc)
    col_idx = col_start[:, None] + jnp.arange(kc)[None, :]
    col_off = col_idx - col[:, None] + (NA_WIN_COLS - 1)

    def row_block(r):
        rs = jnp.clip(r - kr // 2, 0, rows - kr)
        q_r = lax.dynamic_index_in_dim(qg, r, axis=2, keepdims=False)
        k_band = lax.dynamic_slice_in_dim(kg, rs, kr, axis=2)
        v_band = lax.dynamic_slice_in_dim(vg, rs, kr, axis=2)
        k_win = k_band[:, :, :, col_idx]
        v_win = v_band[:, :, :, col_idx]
        row_off = rs + jnp.arange(kr) - r + (NA_WIN_ROWS - 1)
        bias = rpb[:, row_off[None, :, None], col_off[:, None, :]]
        s_loc = jnp.einsum('bhqd,bhrqcd->bhqrc', q_r, k_win).astype(F32) + bias.astype(F32)
        s_ctx = jnp.einsum('bhqd,bhkd->bhqk', q_r, k_ctx).astype(F32)
        s = jnp.concatenate([s_loc.reshape(bsz, NA_HEADS, GRID_W, kr * kc), s_ctx], axis=-1)
        p = jax.nn.softmax(s, axis=-1).astype(v_win.dtype)
        p_loc = p[..., :kr * kc].reshape(bsz, NA_HEADS, GRID_W, kr, kc)
        p_ctx = p[..., kr * kc:]
        return (jnp.einsum('bhqrc,bhrqcd->bhqd', p_loc, v_win)
                + jnp.einsum('bhqk,bhkd->bhqd', p_ctx, v_ctx))

    o = lax.map(row_block, jnp.arange(rows))
    o_lat = o.transpose(1, 0, 3, 2, 4).reshape(bsz, seq_len, NA_WIDTH) @ w_o
    if not need_ctx_out:
        return o_lat, None
    s = jnp.einsum('bhqd,bhkd->bhqk', q_ctx, k_ctx).astype(F32)
    p = jax.nn.softmax(s, axis=-1).astype(v_ctx.dtype)
    o_ctx = jnp.einsum('bhqk,bhkd->bhqd', p, v_ctx).transpose(0, 2, 1, 3).reshape(bsz, n_ctx, NA_WIDTH) @ w_o
    return o_lat, o_ctx


def moe_ffn(h, w_router, b_router, w1, b1, w2, b2):
    n_tok, d = h.shape
    logits = (h @ w_router + b_router).astype(F32)
    top_val, top_idx = lax.top_k(logits, MOE_TOP_K)
    gates = jax.nn.softmax(top_val, axis=-1)
    n_assign = n_tok * MOE_TOP_K
    flat_e = top_idx.reshape(-1)
    order = jnp.argsort(flat_e)
    sorted_e = flat_e[order]
    counts = jnp.bincount(flat_e, length=N_EXPERTS)
    starts = jnp.cumsum(counts) - counts
    padded = (counts + MOE_BLOCK - 1) // MOE_BLOCK * MOE_BLOCK
    padded_ends = jnp.cumsum(padded)
    padded_starts = padded_ends - padded
    dest = padded_starts[sorted_e] + jnp.arange(n_assign) - starts[sorted_e]
    n_blocks = -(-n_assign // MOE_BLOCK) + N_EXPERTS
    n_slots = n_blocks * MOE_BLOCK
    slot_tok = jnp.full((n_slots,), n_tok, jnp.int32).at[dest].set((order // MOE_TOP_K).astype(jnp.int32))
    slot_gate = jnp.zeros((n_slots,), F32).at[dest].set(gates.reshape(-1)[order])
    block_e = jnp.minimum(jnp.searchsorted(padded_ends, jnp.arange(n_blocks) * MOE_BLOCK, side='right'),
                          N_EXPERTS - 1)
    h_pad = jnp.concatenate([h, jnp.zeros((1, d), h.dtype)], axis=0)

    def run_block(args):
        tok, e = args
        xb = h_pad[tok]
        t = xb @ w1[e] + b1[e]
        x_glu = jnp.minimum(t[:, :MOE_FF], SWIGLU_LIMIT)
        x_lin = jnp.clip(t[:, MOE_FF:], -SWIGLU_LIMIT, SWIGLU_LIMIT)
        act = x_glu * jax.nn.sigmoid(SWIGLU_ALPHA * x_glu) * (x_lin + 1)
        return act @ w2[e] + b2[e]

    y = lax.map(run_block, (slot_tok.reshape(n_blocks, MOE_BLOCK), block_e))
    y = y.reshape(n_slots, d) * slot_gate[:, None].astype(y.dtype)
    out = jnp.zeros((n_tok + 1, d), y.dtype).at[slot_tok].add(y)
    return out[:n_tok]


def setup_inputs(seed: int = 0) -> dict:
    key = jax.random.key(seed)
    ks = jax.random.split(key, 32)
    n_even = (DEPTH + 1) // 2
    n_odd = DEPTH // 2
    nrm = jax.random.normal
    d = D_MODEL
    lam_im0 = math.pi * jnp.arange(S5_STATE, dtype=F32)
    decay0 = jnp.asarray(np.log(1.0 - 2.0 ** (-5.0 - np.arange(RET_HEADS))), F32)
    return {
        'x': nrm(ks[0], (BATCH, SEQ, d), F32),
        'c': nrm(ks[1], (BATCH, d), F32),
        'ctx': nrm(ks[2], (BATCH, CTX_LEN, d), F32),
        'c_ctx': nrm(ks[3], (d,), F32),
        'ada_w': nrm(ks[4], (DEPTH, d, 6 * d), F32) * (0.5 * d ** -0.5),
        'ada_b': 0.02 * nrm(ks[5], (DEPTH, 6 * d), F32),
        'norm_w': 1.0 + 0.02 * nrm(ks[6], (DEPTH, 2, d), F32),
        'final_norm_w': 1.0 + 0.02 * nrm(ks[7], (d,), F32),
        'ev_w_in': nrm(ks[8], (n_even, d, EVEN_IN), F32) * d ** -0.5,
        'ev_w_out': nrm(ks[9], (n_even, EVEN_MIX, d), F32) * EVEN_MIX ** -0.5,
        's5_lam_re': -0.5 + 0.01 * nrm(ks[10], (n_even, 2, S5_GROUPS, S5_STATE), F32),
        's5_lam_im': lam_im0 + 0.01 * nrm(ks[11], (n_even, 2, S5_GROUPS, S5_STATE), F32),
        's5_log_step': jax.random.uniform(ks[12], (n_even, 2, S5_GROUPS), F32, math.log(1e-3), math.log(1e-1)),
        's5_b_re': nrm(ks[13], (n_even, 2, S5_GROUPS, S5_STATE, S5_GROUP), F32) * (2 * S5_GROUP) ** -0.5,
        's5_b_im': nrm(ks[14], (n_even, 2, S5_GROUPS, S5_STATE, S5_GROUP), F32) * (2 * S5_GROUP) ** -0.5,
        's5_c_re': nrm(ks[15], (n_even, 2, S5_GROUPS, S5_GROUP, S5_STATE), F32) * (2 * S5_STATE) ** -0.5,
        's5_c_im': nrm(ks[16], (n_even, 2, S5_GROUPS, S5_GROUP, S5_STATE), F32) * (2 * S5_STATE) ** -0.5,
        's5_d': nrm(ks[17], (n_even, S5_WIDTH), F32),
        's5_w_glu': nrm(ks[18], (n_even, S5_WIDTH, S5_WIDTH), F32) * S5_WIDTH ** -0.5,
        'ret_log_decay': decay0 * (1.0 + 0.01 * nrm(ks[19], (n_even, 2, RET_HEADS), F32)),
        'na_w_qkv': nrm(ks[20], (n_odd, d, 3 * NA_WIDTH), F32) * d ** -0.5,
        'na_w_o': nrm(ks[21], (n_odd, NA_WIDTH, d), F32) * NA_WIDTH ** -0.5,
        'na_rpb': 0.02 * nrm(ks[22], (n_odd, NA_HEADS, 2 * NA_WIN_ROWS - 1, 2 * NA_WIN_COLS - 1), F32),
        'moe_w_router': nrm(ks[23], (DEPTH, d, N_EXPERTS), F32) * d ** -0.5,
        'moe_b_router': 0.01 * nrm(ks[24], (DEPTH, N_EXPERTS), F32),
        'moe_w1': nrm(ks[25], (DEPTH, N_EXPERTS, d, 2 * MOE_FF), F32) * d ** -0.5,
        'moe_b1': 0.01 * nrm(ks[26], (DEPTH, N_EXPERTS, 2 * MOE_FF), F32),
        'moe_w2': nrm(ks[27], (DEPTH, N_EXPERTS, MOE_FF, d), F32) * MOE_FF ** -0.5,
        'moe_b2': 0.01 * nrm(ks[28], (DEPTH, N_EXPERTS, d), F32),
    }


def reference(x, c, ctx, c_ctx, ada_w, ada_b, norm_w, final_norm_w, ev_w_in, ev_w_out,
              s5_lam_re, s5_lam_im, s5_log_step, s5_b_re, s5_b_im, s5_c_re, s5_c_im, s5_d, s5_w_glu,
              ret_log_decay, na_w_qkv, na_w_o, na_rpb,
              moe_w_router, moe_b_router, moe_w1, moe_b1, moe_w2, moe_b2):
    bsz, seq_len, d = x.shape
    cond_lat = jax.nn.silu(c)
    cond_ctx = jax.nn.silu(c_ctx)
    for i in range(DEPTH):
        last = i == DEPTH - 1
        sh1, sc1, g1, sh2, sc2, g2 = jnp.split(cond_lat @ ada_w[i] + ada_b[i], 6, axis=-1)
        csh1, csc1, cg1, csh2, csc2, cg2 = jnp.split(cond_ctx @ ada_w[i] + ada_b[i], 6, axis=-1)
        h_lat = modulate(rms_norm(x, norm_w[i, 0]), sh1[:, None], sc1[:, None])
        h_ctx = modulate(rms_norm(ctx, norm_w[i, 0]), csh1, csc1)
        if i % 2 == 0:
            j = i // 2
            m_lat, m_ctx = even_mixer(h_lat, h_ctx, ev_w_in[j], ev_w_out[j],
                                      s5_lam_re[j], s5_lam_im[j], s5_log_step[j], s5_b_re[j], s5_b_im[j],
                                      s5_c_re[j], s5_c_im[j], s5_d[j], s5_w_glu[j], ret_log_decay[j])
        else:
            j = i // 2
            m_lat, m_ctx = na_mixer(h_lat, h_ctx, na_w_qkv[j], na_w_o[j], na_rpb[j], not last)
        x = x + g1[:, None] * m_lat
        h_lat = modulate(rms_norm(x, norm_w[i, 1]), sh2[:, None], sc2[:, None])
        if last:
            y = moe_ffn(h_lat.reshape(-1, d), moe_w_router[i], moe_b_router[i],
                        moe_w1[i], moe_b1[i], moe_w2[i], moe_b2[i])
            x = x + g2[:, None] * y.reshape(bsz, seq_len, d)
        else:
            ctx = ctx + cg1 * m_ctx
            h_ctx = modulate(rms_norm(ctx, norm_w[i, 1]), csh2, csc2)
            tokens = jnp.concatenate([h_lat.reshape(-1, d), h_ctx.reshape(-1, d)], axis=0)
            y = moe_ffn(tokens, moe_w_router[i], moe_b_router[i],
                        moe_w1[i], moe_b1[i], moe_w2[i], moe_b2[i])
            n_lat = bsz * seq_len
            x = x + g2[:, None] * y[:n_lat].reshape(bsz, seq_len, d)
            ctx = ctx + cg2 * y[n_lat:].reshape(ctx.shape)
    return rms_norm(x, final_norm_w)
```

```python
import numpy as np
from contextlib import ExitStack
import concourse.bass as bass
import concourse.mybir as mybir
from concourse.bass_utils import run_bass_kernel_spmd

F32 = mybir.dt.float32
BF16 = mybir.dt.bfloat16
I32 = mybir.dt.int32
AF = mybir.ActivationFunctionType
ALU = mybir.AluOpType
AX = mybir.AxisListType
NCORES = 8


class Ev:
    __slots__ = ("ctr", "cnt", "loop")

    def __init__(self, ctr, cnt, loop):
        self.ctr, self.cnt, self.loop = ctr, cnt, loop


class Ctr:
    def __init__(self, sem, mult):
        self.sem, self.mult, self.cnt = sem, mult, 0


class Loop:
    def __init__(self, n):
        self.n = n
        self.start = {}
        self.per = {}


class Prog:
    CE = ("pe", "act", "dve", "pool")
    RING = 8

    def __init__(self, nc):
        self.nc = nc
        self.es = ExitStack()
        self.q = {e: [] for e in ("pe", "act", "dve", "pool", "sp")}
        self.ctr = {e: Ctr(self.es.enter_context(nc.semaphore("s_" + e)), 1) for e in self.CE}
        self.chans = {}
        self.rings = {}
        self.n = 0
        self.loop = None

    def sb(self, shape, dt=F32, name=None):
        self.n += 1
        return self.es.enter_context(self.nc.sbuf_tensor(name or f"sb{self.n}", list(shape), dt))

    def ps(self, shape, dt=F32, name=None):
        self.n += 1
        return self.es.enter_context(self.nc.psum_tensor(name or f"ps{self.n}", list(shape), dt))

    def _bump(self, c):
        if self.loop is not None and c not in self.loop.start:
            self.loop.start[c] = c.cnt
        c.cnt += 1
        return Ev(c, c.cnt, self.loop)

    def op(self, eng, fn, after=()):
        ev = self._bump(self.ctr[eng])
        self.q[eng].append(("op", fn, tuple(a for a in after if a is not None), ev))
        return ev

    def chan(self, name):
        if name not in self.chans:
            self.chans[name] = Ctr(self.es.enter_context(self.nc.semaphore("c_" + name)), 16)
        return self.chans[name]

    def dma(self, q, out, in_, after=(), ch=None, fn=None, **kw):
        waits = [a for a in after if a is not None]
        if ch is None:
            assert self.loop is None, "ring DMAs are not allowed inside loops; pass ch="
            if q not in self.rings:
                self.rings[q] = [[Ctr(self.es.enter_context(self.nc.semaphore(f"d_{q}{i}")), 16) for i in range(self.RING)], 0]
            ring = self.rings[q]
            i = ring[1]
            ring[1] += 1
            c = ring[0][i % self.RING]
            if c.cnt > 0:
                waits.append(Ev(c, c.cnt, None))
        else:
            c = self.chan(ch)
        ev = self._bump(c)
        f = fn if fn is not None else (lambda e, i=None: e.dma_start(out=out, in_=in_, **kw))
        self.q[q].append(("op", f, tuple(waits), ev))
        return ev

    def loop_begin(self, n):
        assert self.loop is None
        self.loop = Loop(n)
        for qn in self.q:
            self.q[qn].append(("LB", self.loop))

    def loop_end(self):
        L = self.loop
        for c, s in L.start.items():
            L.per[c] = c.cnt - s
            c.cnt = s + L.n * L.per[c]
        for qn in self.q:
            self.q[qn].append(("LE", L))
        self.loop = None

    def emit(self, final_events=()):
        nc = self.nc
        for ev in final_events:
            self.q["sp"].append(("op", None, (ev,), None))
        engmap = {"pe": "tensor", "act": "scalar", "dve": "vector", "pool": "gpsimd", "sp": "sync"}
        loops = []
        for rec in self.q["sp"]:
            if rec[0] == "LB":
                loops.append(rec[1])
        for L in loops:
            L.lsem = {c: self.es.enter_context(nc.semaphore(f"l{self.n}_{k}")) for k, c in enumerate(L.start)}
            L.B = self.es.enter_context(nc.semaphore(f"lB{self.n}"))
            L.G = self.es.enter_context(nc.semaphore(f"lG{self.n}"))
            self.n += 1
        with nc.Block() as block:
            for qn, attr in engmap.items():
                ops = self.q[qn]

                def body(eng, ops=ops, qn=qn):
                    waited = {}
                    lwaited = {}
                    rctx = eng.register("lwreg")
                    rg = rctx.__enter__()
                    cur = None
                    ivar = None
                    ctx = None
                    for rec in ops:
                        if rec[0] == "LB":
                            cur = rec[1]
                            ctx = eng.Fori(0, cur.n)
                            ivar = ctx.__enter__()
                            lwaited = {}
                            continue
                        if rec[0] == "LE":
                            for c, s in cur.start.items():
                                eng.reg_mul(rg, ivar, cur.per[c] * c.mult)
                                eng.reg_add(rg, rg, (s + cur.per[c]) * c.mult)
                                eng.wait_ge(c.sem, rg)
                            ctx.__exit__(None, None, None)
                            for c, s in cur.start.items():
                                waited[id(c)] = (s + cur.n * cur.per[c]) * c.mult
                            cur = None; ivar = None
                            continue
                        _, fn, waits, ev = rec
                        for w in waits:
                            c = w.ctr
                            if w.loop is not None and w.loop is cur:
                                v = w.cnt * c.mult
                                if lwaited.get(id(c), 0) < v:
                                    eng.reg_mul(rg, ivar, cur.per[c] * c.mult)
                                    eng.reg_add(rg, rg, v)
                                    eng.wait_ge(c.sem, rg)
                                    lwaited[id(c)] = v
                                continue
                            v = (w.cnt if w.loop is None else w.cnt + (w.loop.n - 1) * w.loop.per[c]) * c.mult
                            if waited.get(id(c), 0) < v:
                                eng.wait_ge(c.sem, v)
                                waited[id(c)] = v
                        if fn is not None:
                            ins = fn(eng, ivar) if cur is not None else fn(eng)
                            ins.then_inc(ev.ctr.sem, ev.ctr.mult)
                getattr(block, attr)(body)
        self.es.close()


def _run(nc, in_maps):
    res = run_bass_kernel_spmd(nc, in_maps, core_ids=list(range(len(in_maps))))
    return res.results


def build_k0():
    nc = bass.Bass("TRN2", target_bir_lowering=False)
    NCOL = 1536
    condT = nc.dram_tensor("condT", [128, 8, 3], F32, kind="ExternalInput").ap()
    w = nc.dram_tensor("w", [128, 8, NCOL], F32, kind="ExternalInput").ap()
    b = nc.dram_tensor("b", [128, NCOL // 128], F32, kind="ExternalInput").ap()
    out = nc.dram_tensor("out", [128, NCOL // 128, 3], F32, kind="ExternalOutput").ap()
    P = Prog(nc)
    ct_n = NCOL // 128
    c_sb = P.sb([128, 8, 3])
    cs_sb = P.sb([128, 8, 3])
    sg_sb = P.sb([128, 8, 3])
    w_sb = P.sb([128, 8, NCOL])
    b_sb = P.sb([128, ct_n])
    o_sb = P.sb([128, ct_n, 3])
    acc = P.ps([128, 512])
    e_c = P.dma("sp", c_sb[:], condT)
    e_b = P.dma("sp", b_sb[:], b)
    e_w = [P.dma("sp", w_sb[:, kt, :], w[:, kt, :]) for kt in range(8)]
    e_sg = P.op("act", lambda e: e.activation(out=sg_sb[:], in_=c_sb[:], func=AF.Sigmoid), after=[e_c])
    e_s = P.op("dve", lambda e: e.tensor_tensor(out=cs_sb[:], in0=c_sb[:], in1=sg_sb[:], op=ALU.mult), after=[e_sg])
    last = None
    for ct in range(ct_n):
        for kt in range(8):
            last = P.op("pe", lambda e, ct=ct, kt=kt: e.matmul(acc[:, ct * 3:ct * 3 + 3], lhsT=w_sb[:, kt, ct * 128:(ct + 1) * 128],
                                                              rhs=cs_sb[:, kt, :], start=(kt == 0), stop=(kt == 7)),
                        after=[e_s, e_w[kt]] if ct == 0 else [])
    evs = []
    for ct in range(ct_n):
        evs.append(P.op("dve", lambda e, ct=ct: e.tensor_scalar(out=o_sb[:, ct, :], in0=acc[:, ct * 3:ct * 3 + 3], scalar1=b_sb[:, ct:ct + 1],
                                                                 scalar2=None, op0=ALU.add), after=[last, e_b]))
    e_o = P.dma("sp", out, o_sb[:], after=evs)
    P.emit([e_o])
    return nc


def run_k0(c, c_ctx, ada_w, ada_b):
    nc = build_k0()
    cond = np.concatenate([c, c_ctx[None]], 0).astype(np.float32)
    condT = np.ascontiguousarray(cond.T.reshape(8, 128, 3).transpose(1, 0, 2))
    in_maps = []
    for core in range(NCORES):
        l, cc = core // 4, core % 4
        wsl = ada_w[l][:, cc * 1536:(cc + 1) * 1536]
        in_maps.append({
            "condT": condT,
            "w": np.ascontiguousarray(wsl.reshape(8, 128, 1536).transpose(1, 0, 2)),
            "b": np.ascontiguousarray(ada_b[l][cc * 1536:(cc + 1) * 1536].reshape(12, 128).T),
        })
    res = _run(nc, in_maps)
    mods = np.zeros((2, 3, 6144), np.float32)
    for core in range(NCORES):
        l, cc = core // 4, core % 4
        o = res[core]["out"]
        mods[l, :, cc * 1536:(cc + 1) * 1536] = o.transpose(2, 1, 0).reshape(3, 1536)
    return mods


def emit_norm_mod_T(P, xt, nsets, s, A_sb, B_sb, ident, tp_ps, hT, scr, ssq, rstd, xn, after_x, after_free):
    e1 = P.op("act", lambda e: e.activation(out=scr[:], in_=xt[:], func=AF.Square, accum_out=ssq[:]), after=list(after_x) + list(after_free))
    e2 = P.op("act", lambda e: e.activation(out=rstd[:], in_=ssq[:], func=AF.Sqrt, scale=1.0 / 1024.0, bias=EPS_AP[0][:]), after=[e1])
    e3 = P.op("dve", lambda e: e.reciprocal(out=rstd[:], in_=rstd[:]), after=[e2])
    e4 = P.op("dve", lambda e: e.tensor_scalar(out=xn[:], in0=xt[:], scalar1=rstd[:, 0:1], scalar2=None, op0=ALU.mult), after=[e3])
    evs = []
    for half in range(2):
        last = None
        for k in range(4):
            kt = half * 4 + k
            last = P.op("pe", lambda e, kt=kt, k=k, half=half: e.transpose(out=tp_ps[half][:, k * 128:(k + 1) * 128], in_=xn[:, kt * 128:(kt + 1) * 128], identity=ident[:]),
                        after=[e4] + list(after_free))
        for k in range(4):
            kt = half * 4 + k
            evs.append(P.op("act", lambda e, kt=kt, k=k, half=half: e.activation(out=hT[:, kt, :], in_=tp_ps[half][:, k * 128:(k + 1) * 128], func=AF.Identity,
                                                                                 scale=A_sb[:, s, kt:kt + 1], bias=B_sb[:, s, kt:kt + 1]), after=[last]))
    return evs


EPS_AP = [None]


def build_k1(NT, N, tset, has_y=False):
    nc = bass.Bass("TRN2", target_bir_lowering=False)
    x = nc.dram_tensor("x", [NT * 128, 1024], F32, kind="ExternalInput").ap()
    if has_y:
        y_in = nc.dram_tensor("y", [NT * 128, 1024], F32, kind="ExternalInput").ap()
        g2_in = nc.dram_tensor("g2", [128, 2, 1024], F32, kind="ExternalInput").ap()
        xo_d = nc.dram_tensor("xo", [NT * 128, 1024], F32, kind="ExternalOutput").ap()
    nw = nc.dram_tensor("nw", [128, 8], F32, kind="ExternalInput").ap()
    sc = nc.dram_tensor("sc", [128, 2, 8], F32, kind="ExternalInput").ap()
    sh = nc.dram_tensor("sh", [128, 2, 8], F32, kind="ExternalInput").ap()
    w = nc.dram_tensor("w", [128, 8, N], F32, kind="ExternalInput").ap()
    identd = nc.dram_tensor("ident", [128, 128], F32, kind="ExternalInput").ap()
    out = nc.dram_tensor("out", [NT * 128, N], F32, kind="ExternalOutput").ap()
    P = Prog(nc)
    NG = N // 512
    ident = P.sb([128, 128])
    eps = P.sb([128, 1]); EPS_AP[0] = eps
    nw_sb = P.sb([128, 8]); sc_sb = P.sb([128, 2, 8]); B_sb = P.sb([128, 2, 8]); A_sb = P.sb([128, 2, 8])
    w_st = [P.sb([128, N]) for _ in range(2)]
    w_bf = P.sb([128, 8, N], BF16)
    xt = [P.sb([128, 1024]) for _ in range(2)]
    xn = [P.sb([128, 1024]) for _ in range(2)]
    scr = P.sb([128, 1024], BF16)
    ssq = [P.sb([128, 1]) for _ in range(2)]
    rstd = [P.sb([128, 1]) for _ in range(2)]
    hT = [P.sb([128, 8, 128], BF16) for _ in range(2)]
    ot = [P.sb([128, N]) for _ in range(2)]
    tp_ps = [P.ps([128, 512]) for _ in range(2)]
    mm_ps = [P.ps([128, 512]) for _ in range(4)]

    e_id = P.dma("sp", ident[:], identd)
    e_nw = P.dma("sp", nw_sb[:], nw)
    e_sc = P.dma("sp", sc_sb[:], sc)
    e_sh = P.dma("sp", B_sb[:], sh)
    e_eps = P.op("pool", lambda e: e.memset(eps[:], 1e-6))
    e_A = None
    for s in range(2):
        e_A = P.op("dve", lambda e, s=s: e.scalar_tensor_tensor(out=A_sb[:, s, :], in0=sc_sb[:, s, :], scalar=1.0, in1=nw_sb[:], op0=ALU.add, op1=ALU.mult),
                   after=[e_nw, e_sc])
    e_wb = []
    st_free = [None, None]
    for kt in range(8):
        b = kt % 2
        e_l = P.dma("act", w_st[b][:], w[:, kt, :], after=[st_free[b]])
        ev = P.op("pool", lambda e, kt=kt, b=b: e.tensor_copy(out=w_bf[:, kt, :], in_=w_st[b][:]), after=[e_l])
        st_free[b] = ev
        e_wb.append(ev)
    x_free = [None, None]
    y_free = [None, None]; xo_ev = [None, None]; y_free2 = [None, None]
    if has_y:
        yt = [P.sb([128, 1024]) for _ in range(2)]
        g2_sb = P.sb([128, 2, 1024])
        e_g2 = P.dma("sp", g2_sb[:], g2_in)
    h_free = [None, None]
    tp_free = [None, None]
    ot_free = [None, None]
    mm_free = [None] * 4
    outs = []
    mmi = 0
    for t in range(NT):
        b = t % 2
        e_x = P.dma("sp", xt[b][:], x[t * 128:(t + 1) * 128, :], after=[x_free[b], y_free2[b]])
        if has_y:
            e_y = P.dma("sp", yt[b][:], y_in[t * 128:(t + 1) * 128, :], after=[y_free[b]])
            ea = P.op("dve", lambda e, b=b, t=t: e.tensor_tensor(out=yt[b][:], in0=yt[b][:], in1=g2_sb[:, tset[t], :], op=ALU.mult), [e_y, e_g2])
            ea = P.op("dve", lambda e, b=b: e.tensor_tensor(out=xt[b][:], in0=xt[b][:], in1=yt[b][:], op=ALU.add), [ea, e_x])
            e_xo = P.dma("pool", xo_d[t * 128:(t + 1) * 128, :], xt[b][:], after=[ea])
            y_free2[b] = e_xo
            y_free[b] = ea
            e_x = ea
            xo_ev[b] = e_xo
        evs = emit_norm_mod_T(P, xt[b], 2, tset[t], A_sb, B_sb, ident, tp_ps, hT[b], scr, ssq[b], rstd[b], xn[b],
                              after_x=[e_x, e_id, e_A, e_sh, e_eps], after_free=[h_free[b], tp_free[0], tp_free[1]])
        tp_free = [evs[3], evs[7]]
        x_free[b] = evs[-1]
        cps = []
        last_mm = None
        for g in range(NG):
            pb = mmi % 4; mmi += 1
            for kt in range(8):
                last_mm = P.op("pe", lambda e, kt=kt, g=g, pb=pb, b=b: e.matmul(mm_ps[pb][:], lhsT=hT[b][:, kt, :], rhs=w_bf[:, kt, g * 512:(g + 1) * 512],
                                                                                 start=(kt == 0), stop=(kt == 7)),
                               after=(evs + [mm_free[pb], e_wb[kt]]) if kt == 0 or t == 0 else [])
            ce = P.op("dve", lambda e, g=g, pb=pb, b=b: e.tensor_copy(out=ot[b][:, g * 512:(g + 1) * 512], in_=mm_ps[pb][:]), after=[last_mm, ot_free[b]])
            mm_free[pb] = ce
            cps.append(ce)
        h_free[b] = last_mm
        e_o = P.dma("pool", out[t * 128:(t + 1) * 128, :], ot[b][:], after=cps)
        ot_free[b] = e_o
        outs.append(e_o)
    P.emit(outs[-2:] + [e for e in xo_ev if e is not None])
    return nc


def fm_cols(v):
    v = np.asarray(v, np.float32)
    lead = v.shape[:-1]
    return np.ascontiguousarray(np.moveaxis(v.reshape(lead + (8, 128)), -1, 0))


def run_k1(x_tiles, tset, nw, sc2, sh2, W, y_tiles=None, g2s=None):
    NT = x_tiles[0].shape[0] // 128
    N = W.shape[1]
    has_y = y_tiles is not None
    nc = build_k1(NT, N, tset, has_y)
    wl = np.ascontiguousarray(W.reshape(8, 128, N).transpose(1, 0, 2))
    ident = np.eye(128, dtype=np.float32)
    in_maps = [{"x": x_tiles[c], "nw": fm_cols(nw), "sc": fm_cols(sc2[c]), "sh": fm_cols(sh2[c]), "w": wl, "ident": ident} for c in range(NCORES)]
    if has_y:
        for c in range(NCORES):
            in_maps[c]["y"] = y_tiles[c]; in_maps[c]["g2"] = bc_rows(g2s[c])
    res = _run(nc, in_maps)
    if has_y:
        return [res[c]["out"] for c in range(NCORES)], [res[c]["xo"] for c in range(NCORES)]
    return [res[c]["out"] for c in range(NCORES)]


def build_k6(NT):
    nc = bass.Bass("TRN2", target_bir_lowering=False)
    T = NT * 128
    x_d = nc.dram_tensor("x", [T, 1024], F32, kind="ExternalInput").ap()
    y_d = nc.dram_tensor("y", [T, 1024], F32, kind="ExternalInput").ap()
    g2_d = nc.dram_tensor("g2", [128, 1024], F32, kind="ExternalInput").ap()
    fw_d = nc.dram_tensor("fw", [128, 1024], F32, kind="ExternalInput").ap()
    o_d = nc.dram_tensor("out", [T, 1024], F32, kind="ExternalOutput").ap()
    P = Prog(nc)
    g2 = P.sb([128, 1024]); fw = P.sb([128, 1024]); eps = P.sb([128, 1])
    xt = [P.sb([128, 1024]) for _ in range(2)]; yt = [P.sb([128, 1024]) for _ in range(2)]; ot = [P.sb([128, 1024]) for _ in range(2)]
    scr = P.sb([128, 1024], BF16); ssq = P.sb([128, 1]); rstd = P.sb([128, 1])
    c0 = [P.dma("sp", g2[:], g2_d), P.dma("sp", fw[:], fw_d), P.op("pool", lambda e: e.memset(eps[:], 1e-6))]
    xf = [None, None]; yf = [None, None]; of = [None, None]
    outs = []
    prev = None
    for t in range(NT):
        b = t % 2
        rows = slice(t * 128, (t + 1) * 128)
        ex = P.dma("sp", xt[b][:], x_d[rows, :], after=[xf[b]])
        ey = P.dma("act", yt[b][:], y_d[rows, :], after=[yf[b]])
        e = P.op("dve", lambda e_, b=b: e_.tensor_tensor(out=yt[b][:], in0=yt[b][:], in1=g2[:], op=ALU.mult), [ey, prev] + c0)
        e = P.op("dve", lambda e_, b=b: e_.tensor_tensor(out=xt[b][:], in0=xt[b][:], in1=yt[b][:], op=ALU.add), [e, ex])
        yf[b] = e
        e1 = P.op("act", lambda e_, b=b: e_.activation(out=scr[:], in_=xt[b][:], func=AF.Square, accum_out=ssq[:]), [e])
        e2 = P.op("act", lambda e_: e_.activation(out=rstd[:], in_=ssq[:], func=AF.Sqrt, scale=1.0 / 1024.0, bias=eps[:]), [e1])
        e3 = P.op("dve", lambda e_: e_.reciprocal(out=rstd[:], in_=rstd[:]), [e2])
        e4 = P.op("dve", lambda e_, b=b: e_.scalar_tensor_tensor(out=ot[b][:], in0=xt[b][:], scalar=rstd[:, 0:1], in1=fw[:], op0=ALU.mult, op1=ALU.mult), [e3, of[b]])
        xf[b] = e4
        eo = P.dma("pool", o_d[rows, :], ot[b][:], after=[e4])
        of[b] = eo
        prev = e4
        outs.append(eo)
    P.emit(outs[-2:])
    return nc


def run_k6(x_tiles, y_tiles, g2s, fw):
    NT = x_tiles[0].shape[0] // 128
    nc = build_k6(NT)
    in_maps = [{"x": x_tiles[c], "y": y_tiles[c], "g2": bc_rows(g2s[c]), "fw": bc_rows(fw)} for c in range(NCORES)]
    res = _run(nc, in_maps)
    return [res[c]["out"] for c in range(NCORES)]


NTILE = 33
NCHUNK = 130


def build_k2(NTILE=NTILE, NCHUNK=NCHUNK):
    nc = bass.Bass("TRN2", target_bir_lowering=False)
    dt = lambda n, s: nc.dram_tensor(n, s, F32, kind="ExternalInput").ap()
    u_d = dt("u", [NTILE, 128, 512])
    rp_d = dt("rpack", [NCHUNK, 128, 11, 128])
    par_d = dt("par", [128, 3, 8])
    bs_d = dt("bs", [128, 2, 8, 16])
    cs_d = dt("cs", [128, 2, 8, 16])
    cst_d = dt("cst", [128, 4])
    mats_d = dt("mats", [128, 4, 128])
    iota_d = dt("iota1", [128, 128])
    y_d = nc.dram_tensor("y", [NTILE, 128, 512], F32, kind="ExternalOutput").ap()
    o_d = nc.dram_tensor("o", [NCHUNK, 128, 128], F32, kind="ExternalOutput").ap()
    P = Prog(nc)
    par = P.sb([128, 3, 8]); bs = P.sb([128, 2, 8, 16]); cs = P.sb([128, 2, 8, 16]); cst = P.sb([128, 4])
    mats = P.sb([128, 4, 128]); iota1 = P.sb([128, 128])
    ld = [P.dma("sp", a[:], b) for a, b in ((par, par_d), (bs, bs_d), (cs, cs_d), (cst, cst_d), (mats, mats_d), (iota1, iota_d))]
    ident = mats[:, 0, :]; Sw = mats[:, 1, :]
    sgnA = cst[:, 0:1]; sgnB = cst[:, 1:2]; lg = cst[:, 2:3]; rev = cst[:, 3:4]
    SC = 128.0 ** -0.5

    def T8():
        return P.sb([128, 8])
    dve = lambda fn, after: P.op("dve", fn, after=after)
    act = lambda fn, after: P.op("act", fn, after=after)
    lr = T8(); step = T8(); ex = T8(); mag = T8(); ang = T8(); s16 = T8(); tmp = T8(); tmp2 = T8()
    CK = [T8() for _ in range(10)]; SK = [T8() for _ in range(10)]
    c8 = T8(); s8 = T8(); c4 = T8(); s4 = T8(); c2 = T8(); s2 = T8()
    e = dve(lambda e_: e_.tensor_scalar(out=lr[:], in0=par[:, 0, :], scalar1=-1e-4, scalar2=None, op0=ALU.min), ld)
    e = act(lambda e_: e_.activation(out=step[:], in_=par[:, 2, :], func=AF.Exp), [e])
    e = dve(lambda e_: e_.tensor_tensor(out=ex[:], in0=lr[:], in1=step[:], op=ALU.mult), [e])
    e_mag = act(lambda e_: e_.activation(out=mag[:], in_=ex[:], func=AF.Exp), [e])
    e = dve(lambda e_: e_.tensor_tensor(out=ang[:], in0=par[:, 1, :], in1=step[:], op=ALU.mult), [e_mag])
    e1 = act(lambda e_: e_.activation(out=s16[:], in_=ang[:], func=AF.Sin, scale=1.0 / 16.0), [e])
    e2 = act(lambda e_: e_.activation(out=s8[:], in_=ang[:], func=AF.Sin, scale=1.0 / 8.0), [e1])
    e = dve(lambda e_: e_.tensor_tensor(out=tmp[:], in0=s16[:], in1=s16[:], op=ALU.mult), [e2])
    e = dve(lambda e_: e_.tensor_scalar(out=c8[:], in0=tmp[:], scalar1=-2.0, scalar2=1.0, op0=ALU.mult, op1=ALU.add), [e])

    def square(ci, si, co, so, e):
        e = dve(lambda e_: e_.tensor_tensor(out=tmp[:], in0=si[:], in1=si[:], op=ALU.mult), [e])
        e = dve(lambda e_: e_.tensor_tensor(out=tmp2[:], in0=ci[:], in1=ci[:], op=ALU.mult), [e])
        e = dve(lambda e_: e_.tensor_tensor(out=so[:], in0=ci[:], in1=si[:], op=ALU.mult), [e])
        e = dve(lambda e_: e_.tensor_scalar(out=so[:], in0=so[:], scalar1=2.0, scalar2=None, op0=ALU.mult), [e])
        e = dve(lambda e_: e_.tensor_tensor(out=co[:], in0=tmp2[:], in1=tmp[:], op=ALU.subtract), [e])
        return e
    e = square(c8, s8, c4, s4, e)
    e = square(c4, s4, c2, s2, e)
    e = square(c2, s2, CK[0], SK[0], e)
    for k in range(9):
        e = square(CK[k], SK[k], CK[k + 1], SK[k + 1], e)
    are = T8(); aim = T8(); den = T8(); nre = T8(); core_ = T8(); coim = T8(); coimA = T8(); coreB = T8(); skB = T8()
    e = dve(lambda e_: e_.tensor_tensor(out=are[:], in0=mag[:], in1=CK[0][:], op=ALU.mult), [e])
    e = dve(lambda e_: e_.tensor_tensor(out=aim[:], in0=mag[:], in1=SK[0][:], op=ALU.mult), [e])
    e = dve(lambda e_: e_.tensor_tensor(out=tmp[:], in0=lr[:], in1=lr[:], op=ALU.mult), [e])
    e = dve(lambda e_: e_.tensor_tensor(out=den[:], in0=par[:, 1, :], in1=par[:, 1, :], op=ALU.mult), [e])
    e = dve(lambda e_: e_.tensor_tensor(out=den[:], in0=den[:], in1=tmp[:], op=ALU.add), [e])
    e = dve(lambda e_: e_.reciprocal(out=den[:], in_=den[:]), [e])
    e = dve(lambda e_: e_.tensor_scalar(out=nre[:], in0=are[:], scalar1=-1.0, scalar2=None, op0=ALU.add), [e])
    e = dve(lambda e_: e_.tensor_tensor(out=tmp[:], in0=nre[:], in1=lr[:], op=ALU.mult), [e])
    e = dve(lambda e_: e_.tensor_tensor(out=tmp2[:], in0=aim[:], in1=par[:, 1, :], op=ALU.mult), [e])
    e = dve(lambda e_: e_.tensor_tensor(out=tmp[:], in0=tmp[:], in1=tmp2[:], op=ALU.add), [e])
    e = dve(lambda e_: e_.tensor_tensor(out=core_[:], in0=tmp[:], in1=den[:], op=ALU.mult), [e])
    e = dve(lambda e_: e_.tensor_tensor(out=tmp[:], in0=aim[:], in1=lr[:], op=ALU.mult), [e])
    e = dve(lambda e_: e_.tensor_tensor(out=tmp2[:], in0=nre[:], in1=par[:, 1, :], op=ALU.mult), [e])
    e = dve(lambda e_: e_.tensor_tensor(out=tmp[:], in0=tmp[:], in1=tmp2[:], op=ALU.subtract), [e])
    e = dve(lambda e_: e_.tensor_tensor(out=coim[:], in0=tmp[:], in1=den[:], op=ALU.mult), [e])
    e = dve(lambda e_: e_.tensor_scalar(out=coimA[:], in0=coim[:], scalar1=sgnA, scalar2=None, op0=ALU.mult), [e])
    e = dve(lambda e_: e_.tensor_scalar(out=coreB[:], in0=core_[:], scalar1=sgnB, scalar2=None, op0=ALU.mult), [e])
    e = dve(lambda e_: e_.tensor_scalar(out=skB[:], in0=SK[9][:], scalar1=sgnB, scalar2=None, op0=ALU.mult), [e])
    Bw = [P.sb([128, 128]) for _ in range(2)]
    t16 = P.sb([128, 16])
    L1 = P.sb([128, 8, 128], BF16); L2 = P.sb([128, 8, 128], BF16)
    C1 = P.sb([128, 8, 128], BF16); C2 = P.sb([128, 8, 128], BF16)
    KB = P.sb([128, 8, 128])
    Rt = P.sb([128, 8, 512]); Tc = P.sb([128, 8, 512]); Ts = P.sb([128, 8, 512])
    tt = P.sb([128, 256]); tt2 = P.sb([128, 256])
    banks = [P.ps([128, 512]) for _ in range(7)]
    e_z = P.op("pool", lambda e_: e_.memset(Bw[0][:], 0.0))
    e_z = P.op("pool", lambda e_: e_.memset(Bw[1][:], 0.0), [e_z])
    e_z = P.op("pool", lambda e_: e_.memset(C1[:], 0.0), [e_z])
    e_z = P.op("pool", lambda e_: e_.memset(C2[:], 0.0), [e_z])
    e_z = P.op("pool", lambda e_: e_.memset(Rt[:], 1.0), [e_z])
    e_z = P.op("pool", lambda e_: e_.memset(Tc[:], 1.0), [e_z])
    e_z = P.op("pool", lambda e_: e_.memset(Ts[:], 0.0), [e_z])
    e = dve(lambda e_: e_.tensor_copy(out=tmp[:], in_=tmp[:]), [e, e_z])
    def setup_group(g, e):
        cg = slice(16 * g, 16 * g + 16)
        for v, (sa, sb_, Lx) in enumerate((((core_, 0), (coimA, 1), L1), ((coim, 0), (coreB, 1), L2))):
            (s_a, ia), (s_b, ib) = sa, sb_
            e = dve(lambda e_, s_a=s_a, ia=ia: e_.tensor_scalar(out=t16[:], in0=bs[:, ia, g, :], scalar1=s_a[:, g:g + 1], scalar2=None, op0=ALU.mult), [e])
            e = dve(lambda e_, s_b=s_b, ib=ib, v=v: e_.scalar_tensor_tensor(out=Bw[v][:, cg], in0=bs[:, ib, g, :], scalar=s_b[:, g:g + 1], in1=t16[:],
                                                                           op0=ALU.mult, op1=ALU.add), [e])
            ep = P.op("pe", lambda e_, v=v: e_.transpose(out=banks[v][:, 0:128], in_=Bw[v][:], identity=ident), [e])
            e = dve(lambda e_, v=v, Lx=Lx: e_.tensor_copy(out=Lx[:, g, :], in_=banks[v][:, 0:128]), [ep])
            e = dve(lambda e_, v=v: e_.memset(Bw[v][:, cg], 0.0), [e])
        e = dve(lambda e_: e_.tensor_scalar(out=C1[:, g, cg], in0=cs[:, 0, g, :], scalar1=sgnB, scalar2=None, op0=ALU.mult), [e])
        e = dve(lambda e_: e_.tensor_scalar(out=C2[:, g, cg], in0=cs[:, 1, g, :], scalar1=-1.0, scalar2=None, op0=ALU.mult), [e])
        e = dve(lambda e_: e_.tensor_scalar(out=KB[:, g, :], in0=Sw, scalar1=skB[:, g:g + 1], scalar2=None, op0=ALU.mult), [e])
        e = dve(lambda e_: e_.scalar_tensor_tensor(out=KB[:, g, :], in0=ident, scalar=CK[9][:, g:g + 1], in1=KB[:, g, :], op0=ALU.mult, op1=ALU.add), [e])
        e = dve(lambda e_: e_.tensor_scalar(out=Rt[:, g, :], in0=Rt[:, g, :], scalar1=mag[:, g:g + 1], scalar2=None, op0=ALU.mult), [e])
        for k in range(9):
            n = 1 << k
            ck = CK[k][:, g:g + 1]; sk = SK[k][:, g:g + 1]
            e = dve(lambda e_, n=n, sk=sk: e_.tensor_scalar(out=tt[:, 0:n], in0=Ts[:, g, 0:n], scalar1=sk, scalar2=None, op0=ALU.mult), [e])
            e = dve(lambda e_, n=n, ck=ck: e_.tensor_scalar(out=tt2[:, 0:n], in0=Ts[:, g, 0:n], scalar1=ck, scalar2=None, op0=ALU.mult), [e])
            e = dve(lambda e_, n=n, ck=ck: e_.scalar_tensor_tensor(out=Tc[:, g, n:2 * n], in0=Tc[:, g, 0:n], scalar=ck, in1=tt[:, 0:n], op0=ALU.mult, op1=ALU.subtract), [e])
            e = dve(lambda e_, n=n, sk=sk: e_.scalar_tensor_tensor(out=Ts[:, g, n:2 * n], in0=Tc[:, g, 0:n], scalar=sk, in1=tt2[:, 0:n], op0=ALU.mult, op1=ALU.add), [e])
        return e
    for g in range(8):
        e = setup_group(g, e)
    e_setup = e
    Mask = P.sb([128, 128]); Dq = P.sb([128, 128]); kdec = P.sb([128, 1]); g128 = P.sb([128, 1])
    ea = act(lambda e_: e_.activation(out=Mask[:], in_=mats[:, 2, :], func=AF.Exp, scale=lg), ld)
    ea = dve(lambda e_: e_.scalar_tensor_tensor(out=Mask[:], in0=Mask[:], scalar=SC, in1=mats[:, 3, :], op0=ALU.mult, op1=ALU.mult), [ea, e_setup])
    eb = act(lambda e_: e_.activation(out=Dq[:], in_=iota1[:], func=AF.Exp, scale=lg), [ea])
    eb = act(lambda e_: e_.activation(out=kdec[:], in_=rev, func=AF.Exp, scale=lg), [eb])
    eb = act(lambda e_: e_.activation(out=g128[:], in_=iota1[:, 127:128], func=AF.Exp, scale=lg), [eb])
    e_rc = dve(lambda e_: e_.tensor_scalar(out=kdec[:], in0=kdec[:], scalar1=SC, scalar2=None, op0=ALU.mult), [eb, ea])

    uf = [P.sb([128, 512]) for _ in range(2)]; ub = [P.sb([128, 512], BF16) for _ in range(2)]
    t1 = [P.sb([128, 512]) for _ in range(2)]; zz = [P.sb([128, 512]) for _ in range(2)]
    ww = [[P.sb([128, 512]) for _ in range(2)] for _ in range(8)]
    H1 = [P.sb([128, 512], BF16) for _ in range(8)]; H2 = [P.sb([128, 512], BF16) for _ in range(8)]
    winit = P.sb([128, 8]); yo = [P.sb([128, 512]) for _ in range(2)]
    rp = [P.sb([128, 11, 128]) for _ in range(2)]
    qr = [P.sb([128, 128], BF16) for _ in range(2)]; kr = [P.sb([128, 128], BF16) for _ in range(2)]; qd = [P.sb([128, 128], BF16) for _ in range(2)]
    kt_ = [P.sb([128, 128], BF16) for _ in range(2)]; vb = [P.sb([128, 128], BF16) for _ in range(2)]
    am = [P.sb([128, 128], BF16) for _ in range(2)]
    ra = P.sb([128, 128]); rb = P.sb([128, 128]); rc = P.sb([128, 128]); rd = P.sb([128, 128]); re_ = P.sb([128, 128]); rf = P.sb([128, 128])
    S = P.sb([128, 128]); Sb = [P.sb([128, 128], BF16) for _ in range(2)]
    oo = [P.sb([128, 128]) for _ in range(2)]
    e_s0 = P.op("pool", lambda e_: e_.memset(S[:], 0.0), [e_z])
    e_s0 = P.op("pool", lambda e_: e_.memset(Sb[0][:], 0.0), [e_s0])
    e_w0 = P.op("pool", lambda e_: e_.memset(winit[:], 0.0), [e_s0])
    PA = [banks[0], banks[2]]; PB = [banks[1], banks[3]]; YB = banks[4]; OB = banks[5]; MB = banks[6]
    pfree = [e_setup, e_setup]
    tz_free = [None, None]; h_free = [None, None]; uf_free = [None, None]; ub_free = [None, None]; yo_free = [None, None]
    w_prev = [e_w0] * 8
    w_free = [[None, None] for _ in range(8)]
    rp_free = [None, None]; oo_free = [None, None]
    outs = []
    gi = 0
    e_S = e_s0; e_Sb = e_s0; sb_free = [None, None]
    y_free = None; ob_free = None; mb_att_free = None; mb_ds_free = None
    qk_free = [None, None]; am_free = [None, None]; kt_free = [None, None]
    ub_ev = {}
    eh_ev = {}
    hy_free = [None]

    def load_u(n):
        b = n % 2
        e_u = P.dma("sp", uf[b][:], u_d[n], after=[uf_free[b]])
        e_ub = P.op("pool", lambda e_, b=b: e_.tensor_copy(out=ub[b][:], in_=uf[b][:]), [e_u, ub_free[b]])
        uf_free[b] = e_ub
        ub_ev[n] = e_ub

    ep_ev = {}

    def pe_load(n, g):
        b = n % 2; pb = g % 2
        ep1 = P.op("pe", lambda e_: e_.matmul(PA[pb][:], lhsT=L1[:, g, :], rhs=ub[b][:], start=True, stop=True), [ub_ev[n], pfree[pb]])
        ep2 = P.op("pe", lambda e_: e_.matmul(PB[pb][:], lhsT=L2[:, g, :], rhs=ub[b][:], start=True, stop=True), [])
        ep_ev[(n, g)] = (ep1, ep2)
        if g == 7:
            ub_free[b] = ep2

    load_u(0)
    pe_load(0, 0); pe_load(0, 1)
    for n in range(NTILE):
        b = n % 2
        if n + 1 < NTILE:
            load_u(n + 1)
        last_y = None
        for gp in range(4):
            gs = (2 * gp, 2 * gp + 1)
            ea2s = {}; adds = {}; scans = {}
            for g in gs:
                pb = g % 2
                ep1, ep2 = ep_ev[(n, g)]
                ea1 = dve(lambda e_, g=g, pb=pb: e_.tensor_tensor(out=t1[pb][:], in0=PA[pb][:], in1=Tc[:, g, :], op=ALU.mult), [ep1, tz_free[pb]])
                ea2 = dve(lambda e_, g=g, pb=pb: e_.tensor_tensor(out=zz[pb][:], in0=PB[pb][:], in1=Ts[:, g, :], op=ALU.mult), [ep2])
                pfree[pb] = ea2
                ea2s[g] = (ea1, ea2)
            for g in gs:
                pb = g % 2
                adds[g] = dve(lambda e_, pb=pb: e_.tensor_tensor(out=zz[pb][:], in0=zz[pb][:], in1=t1[pb][:], op=ALU.add), list(ea2s[g]))
            for g in gs:
                pb = g % 2
                esc = dve(lambda e_, g=g, pb=pb, b=b: e_.tensor_tensor_scan(out=ww[g][b][:], data0=Rt[:, g, :], data1=zz[pb][:], initial=winit[:, g:g + 1],
                                                                           op0=ALU.mult, op1=ALU.add), [adds[g], w_prev[g], w_free[g][b]])
                tz_free[pb] = esc
                scans[g] = esc
            nxt = [(n, g + 2) for g in gs] if gp < 3 else ([(n + 1, 0), (n + 1, 1)] if n + 1 < NTILE else [])
            for (nn, gg) in nxt:
                pe_load(nn, gg)
            for g in gs:
                pb = g % 2
                esc = scans[g]
                ec = P.op("pe", lambda e_, g=g, b=b: e_.matmul(MB[:, g:g + 1], lhsT=KB[:, g, :], rhs=ww[g][b][:, 511:512], start=True, stop=True), [esc])
                w_prev[g] = act(lambda e_, g=g: e_.activation(out=winit[:, g:g + 1], in_=MB[:, g:g + 1], func=AF.Copy), [ec])
                eh1 = P.op("pool", lambda e_, g=g, pb=pb, b=b: e_.tensor_tensor(out=H1[g][:], in0=ww[g][b][:], in1=Tc[:, g, :], op=ALU.mult), [esc, hy_free[0]])
                eh2 = P.op("pool", lambda e_, g=g, pb=pb, b=b: e_.tensor_tensor(out=H2[g][:], in0=ww[g][b][:], in1=Ts[:, g, :], op=ALU.mult), [esc])
                eh_ev[g] = (eh1, eh2)
                w_free[g][b] = eh2
        for g in range(8):
            ey = P.op("pe", lambda e_, g=g: e_.matmul(YB[:], lhsT=C1[:, g, :], rhs=H1[g][:], start=(g == 0), stop=False), [eh_ev[g][0], eh_ev[g][1], y_free])
            ey = P.op("pe", lambda e_, g=g: e_.matmul(YB[:], lhsT=C2[:, g, :], rhs=H2[g][:], start=False, stop=(g == 7)), [])
            last_y = ey
        hy_free[0] = last_y
        ecp = act(lambda e_, b=b: e_.activation(out=yo[b][:], in_=YB[:], func=AF.Copy), [last_y, yo_free[b]])
        y_free = ecp
        e_o = P.dma("pool", y_d[n], yo[b][:], after=[ecp])
        yo_free[b] = e_o
        outs.append(e_o)
        for c in range(4 * n, min(4 * n + 4, NCHUNK)):
            cb = c % 2
            e_r = P.dma("act", rp[cb][:], rp_d[c], after=[rp_free[cb]])
            R = rp[cb]
            x1 = dve(lambda e_, R=R: e_.tensor_tensor(out=ra[:], in0=R[:, 0, :], in1=R[:, 4, :], op=ALU.mult), [e_r, e_rc])
            x2 = dve(lambda e_, R=R: e_.tensor_tensor(out=rb[:], in0=R[:, 1, :], in1=R[:, 5, :], op=ALU.mult), [x1])
            x3 = dve(lambda e_, cb=cb: e_.tensor_tensor(out=rc[:], in0=ra[:], in1=rb[:], op=ALU.add), [x2])
            x3b = P.op("pool", lambda e_, cb=cb: e_.tensor_copy(out=qr[cb][:], in_=rc[:]), [x3, qk_free[cb]])
            x4 = P.op("pool", lambda e_, cb=cb: e_.tensor_tensor(out=qd[cb][:], in0=rc[:], in1=Dq[:], op=ALU.mult), [x3b])
            x5 = dve(lambda e_, R=R: e_.tensor_tensor(out=ra[:], in0=R[:, 2, :], in1=R[:, 4, :], op=ALU.mult), [x4])
            x6 = dve(lambda e_, R=R: e_.tensor_tensor(out=rb[:], in0=R[:, 3, :], in1=R[:, 5, :], op=ALU.mult), [x5])
            x7 = dve(lambda e_, cb=cb: e_.tensor_tensor(out=kr[cb][:], in0=ra[:], in1=rb[:], op=ALU.add), [x6, qk_free[cb]])
            x8 = dve(lambda e_, R=R: e_.tensor_tensor(out=rd[:], in0=R[:, 6, :], in1=R[:, 8, :], op=ALU.mult), [x7])
            x9 = dve(lambda e_, R=R: e_.tensor_tensor(out=re_[:], in0=R[:, 7, :], in1=R[:, 9, :], op=ALU.mult), [x8])
            x10 = dve(lambda e_: e_.tensor_tensor(out=rf[:], in0=rd[:], in1=re_[:], op=ALU.add), [x9])
            x11 = act(lambda e_, cb=cb: e_.activation(out=kt_[cb][:], in_=rf[:], func=AF.Copy, scale=kdec[:, 0:1]), [x10, kt_free[cb]])
            x12 = act(lambda e_, cb=cb, R=R: e_.activation(out=vb[cb][:], in_=R[:, 10, :], func=AF.Copy), [x11])
            rp_free[cb] = x12
            m1 = P.op("pe", lambda e_, cb=cb: e_.matmul(MB[:, 256:384], lhsT=kr[cb][:], rhs=qr[cb][:], start=True, stop=True), [x7, x3b, mb_att_free])
            x13 = dve(lambda e_, cb=cb: e_.tensor_tensor(out=am[cb][:], in0=MB[:, 256:384], in1=Mask[:], op=ALU.mult), [m1, am_free[cb]])
            mb_att_free = x13
            m2 = P.op("pe", lambda e_, cb=cb: e_.matmul(OB[:, 0:128], lhsT=am[cb][:], rhs=vb[cb][:], start=True, stop=False), [x13, x12, ob_free])
            m3 = P.op("pe", lambda e_, cb=cb: e_.matmul(OB[:, 0:128], lhsT=qd[cb][:], rhs=Sb[cb][:], start=False, stop=True), [x4, e_Sb])
            am_free[cb] = m3
            qk_free[cb] = m3
            m4 = P.op("pe", lambda e_, cb=cb: e_.matmul(MB[:, 128:256], lhsT=kt_[cb][:], rhs=vb[cb][:], start=True, stop=True), [x11, x12, mb_ds_free])
            kt_free[cb] = m4
            x14 = act(lambda e_, cb=cb: e_.activation(out=oo[cb][:], in_=OB[:, 0:128], func=AF.Copy), [m3, oo_free[cb]])
            ob_free = x14
            e_S = dve(lambda e_: e_.scalar_tensor_tensor(out=S[:], in0=S[:], scalar=g128[:, 0:1], in1=MB[:, 128:256], op0=ALU.mult, op1=ALU.add), [m4, e_S])
            mb_ds_free = e_S
            e_Sb = act(lambda e_, cb=cb: e_.activation(out=Sb[1 - cb][:], in_=S[:], func=AF.Copy), [e_S, m3])
            e_ro = P.dma("pool", o_d[c], oo[cb][:], after=[x14])
            oo_free[cb] = e_ro
            outs.append(e_ro)
    P.emit(outs[-6:])
    return nc


def _rope_tables():
    half = 32
    freq = (10000.0 ** (-np.arange(half, dtype=np.float32) / half)).astype(np.float32)
    t = np.arange(16384)
    row = (t // 64).astype(np.float32); col = (t % 64).astype(np.float32)
    cos = np.ones((16640, 128), np.float32); sin = np.zeros((16640, 128), np.float32)
    for off, pos in ((0, row), (64, col)):
        ang = pos[:, None] * freq[None, :]
        c, s = np.cos(ang).astype(np.float32), np.sin(ang).astype(np.float32)
        cos[256:, off:off + 32] = c; cos[256:, off + 32:off + 64] = c
        sin[256:, off:off + 32] = -s; sin[256:, off + 32:off + 64] = s
    return cos, sin


def _rot_perm():
    idx = np.arange(128).reshape(2, 2, 32)[:, ::-1, :].reshape(128)
    return idx


def run_k2(p_lat, p_ctx, prm, d, NTILE=NTILE, NCHUNK=NCHUNK):
    nc = build_k2(NTILE, NCHUNK)
    cos, sin = _rope_tables()
    perm = _rot_perm()
    T = NCHUNK * 128
    seq = np.concatenate([p_ctx, p_lat], axis=1)
    if d == 1:
        seq = np.concatenate([p_ctx[:, ::-1], p_lat[:, ::-1]], axis=1)
        cos = np.concatenate([cos[:256][::-1], cos[256:][::-1]]); sin = np.concatenate([sin[:256][::-1], sin[256:][::-1]])
    seq = seq[:, :T]; cos = cos[:T]; sin = sin[:T]
    ident = np.eye(128, dtype=np.float32)
    Sw = np.roll(ident, 64, axis=1)
    ii = np.arange(128)
    D = np.maximum(ii[None, :] - ii[:, None], 0).astype(np.float32)
    tri = (ii[None, :] >= ii[:, None]).astype(np.float32)
    mats = np.ascontiguousarray(np.stack([ident, Sw, D, tri], 1))
    iota1 = np.broadcast_to((ii + 1).astype(np.float32)[None, :], (128, 128)).copy()
    sgnA = np.concatenate([-np.ones(64), np.ones(64)]).astype(np.float32)
    in_maps = []
    for core in range(NCORES):
        b, j = core // 4, core % 4
        gs = slice(8 * j, 8 * j + 8)
        u = seq[b, :, 128 * j:128 * j + 128]
        upad = np.zeros((NTILE * 512, 128), np.float32); upad[:T] = u
        u_fm = np.ascontiguousarray(upad.reshape(NTILE, 512, 128).transpose(0, 2, 1))
        q = seq[b, :, 512 + 128 * j:512 + 128 * j + 128]; k = seq[b, :, 1024 + 128 * j:1024 + 128 * j + 128]
        v = seq[b, :, 1536 + 128 * j:1536 + 128 * j + 128]
        def fm(a):
            return a.reshape(NCHUNK, 128, 128).transpose(0, 2, 1)
        def tm(a):
            return a.reshape(NCHUNK, 128, 128)
        rpack = np.stack([fm(q), fm(q[:, perm]), fm(k), fm(k[:, perm]), fm(cos), fm(sin),
                          tm(k), tm(k[:, perm]), tm(cos), tm(sin), tm(v)], axis=2)
        lre = prm['s5_lam_re'][0, d, gs]; lim = prm['s5_lam_im'][0, d, gs]; lst = prm['s5_log_step'][0, d, gs]
        par = np.stack([np.concatenate([lre.T, lre.T]), np.concatenate([lim.T, lim.T]), np.broadcast_to(lst[None, :], (128, 8))], 1)
        bre = prm['s5_b_re'][0, d, gs].transpose(1, 0, 2); bim = prm['s5_b_im'][0, d, gs].transpose(1, 0, 2)
        bs = np.stack([np.concatenate([bre, bim]), np.concatenate([bim, bre])], 1)
        cre = prm['s5_c_re'][0, d, gs].transpose(2, 0, 1); cim = prm['s5_c_im'][0, d, gs].transpose(2, 0, 1)
        csm = np.stack([np.concatenate([cre, cim]), np.concatenate([cim, cre])], 1)
        lg = np.full(128, prm['ret_log_decay'][0, d, j], np.float32)
        cst = np.stack([sgnA, -sgnA, lg, (127 - ii).astype(np.float32)], 1)
        in_maps.append({"u": u_fm, "rpack": np.ascontiguousarray(rpack, dtype=np.float32), "par": np.ascontiguousarray(par, dtype=np.float32),
                        "bs": np.ascontiguousarray(bs, dtype=np.float32), "cs": np.ascontiguousarray(csm, dtype=np.float32),
                        "cst": np.ascontiguousarray(cst, dtype=np.float32), "mats": mats, "iota1": iota1})
    res = _run(nc, in_maps)
    y = np.zeros((2, T, 512), np.float32); o = np.zeros((2, T, 512), np.float32)
    for core in range(NCORES):
        b, j = core // 4, core % 4
        yy = res[core]["y"].transpose(0, 2, 1).reshape(NTILE * 512, 128)[:T]
        y[b, :, 128 * j:128 * j + 128] = yy
        o[b, :, 128 * j:128 * j + 128] = res[core]["o"].reshape(T, 128)
    return y, o


def build_k3(NT, tset, even):
    nc = bass.Bass("TRN2", target_bir_lowering=False)
    T = NT * 128
    dt = lambda n, s: nc.dram_tensor(n, s, F32, kind="ExternalInput").ap()
    x_d = dt("x", [T, 1024])
    if even:
        s5_d = dt("s5in", [T, 3, 512])
        rt_d = dt("retin", [T, 3, 512])
        drow_d = dt("drow", [128, 512])
        wglu_d = dt("wglu", [128, 4, 512])
    else:
        a_d = dt("a", [T, 1024])
    w_d = dt("w", [128, 8, 1024])
    g1_d = dt("g1", [128, 2, 1024])
    nw_d = dt("nw", [128, 8]); sc_d = dt("sc", [128, 2, 8]); sh_d = dt("sh", [128, 2, 8])
    wr_d = dt("wr", [128, 8, 32]); br_d = dt("br", [128, 32])
    id_d = dt("ident", [128, 128])
    x1_d = nc.dram_tensor("x1", [T, 1024], F32, kind="ExternalOutput").ap()
    h2_d = nc.dram_tensor("h2T", [8, 128, T], F32, kind="ExternalOutput").ap()
    gt_d = nc.dram_tensor("gates", [T, 32], F32, kind="ExternalOutput").ap()
    P = Prog(nc)
    ident = P.sb([128, 128]); identb = P.sb([128, 128], BF16)
    eps = P.sb([128, 1]); EPS_AP[0] = eps
    w_bf = P.sb([128, 8, 1024], BF16); g1 = P.sb([128, 2, 1024])
    nw_sb = P.sb([128, 8]); sc_sb = P.sb([128, 2, 8]); B_sb = P.sb([128, 2, 8]); A_sb = P.sb([128, 2, 8])
    wr = P.sb([128, 8, 32]); br = P.sb([128, 32])
    cl = [P.dma("sp", a[:], b) for a, b in ((ident, id_d), (g1, g1_d), (nw_sb, nw_d), (sc_sb, sc_d), (B_sb, sh_d), (wr, wr_d), (br, br_d))]
    cl.append(P.dma("pool", w_bf[:], w_d))
    if even:
        drow = P.sb([128, 512]); wglu = P.sb([128, 4, 512], BF16)
        cl.append(P.dma("sp", drow[:], drow_d)); cl.append(P.dma("pool", wglu[:], wglu_d))
    e0 = P.op("pool", lambda e: e.memset(eps[:], 1e-6))
    e0 = P.op("pool", lambda e: e.tensor_copy(out=identb[:], in_=ident[:]), cl + [e0])
    for s in range(2):
        e0 = P.op("dve", lambda e, s=s: e.scalar_tensor_tensor(out=A_sb[:, s, :], in0=sc_sb[:, s, :], scalar=1.0, in1=nw_sb[:], op0=ALU.add, op1=ALU.mult), [e0])
    e_c = e0
    banks = [P.ps([128, 512]) for _ in range(8)]
    tpb = banks[0]
    tpb_bf = tpb[:].bitcast(BF16)
    xt = [P.sb([128, 1024]) for _ in range(2)]
    if even:
        s5t = [P.sb([128, 3, 512]) for _ in range(2)]; rtt = [P.sb([128, 3, 512]) for _ in range(2)]
        ya = P.sb([128, 512]); yb2 = P.sb([128, 512]); ysg = P.sb([128, 512]); a_bf = P.sb([128, 512], BF16)
        aT = P.sb([128, 4, 128], BF16); zsg = P.sb([128, 512])
        osum = P.sb([128, 512]); gsil = P.sb([128, 512]); ssq4 = P.sb([128, 4]); scr = P.sb([128, 512], BF16)
    else:
        at = [P.sb([128, 1024]) for _ in range(2)]
    A_bf = P.sb([128, 1024], BF16); AT = P.sb([128, 8, 128], BF16)
    mt = P.sb([128, 1024]); x1 = [P.sb([128, 1024]) for _ in range(2)]
    scr2 = P.sb([128, 1024], BF16); ssq = P.sb([128, 1]); rstd = P.sb([128, 1]); xn = P.sb([128, 1024])
    hT = [P.sb([128, 8, 128]) for _ in range(2)]
    lgt = P.sb([128, 32]); m8 = P.sb([128, 8]); msk = P.sb([128, 32]); nmx = P.sb([128, 1]); ex = P.sb([128, 32]); ssum = P.sb([128, 1])
    gts = [P.sb([128, 32]) for _ in range(2)]
    prev = e_c
    x_free = [None, None]; in_free = [None, None]; x1_free = [None, None]; h_free = [None, None]; g_free = [None, None]
    outs = []
    for t in range(NT):
        b = t % 2; s = tset[t]
        rows = slice(t * 128, (t + 1) * 128)
        e_x = P.dma("sp", xt[b][:], x_d[rows, :], after=[x_free[b]])
        if even:
            e_i1 = P.dma("sp", s5t[b][:], s5_d[rows], after=[in_free[b]])
            e_i2 = P.dma("act", rtt[b][:], rt_d[rows], after=[in_free[b]])
            S5, RT = s5t[b], rtt[b]
            e = P.op("dve", lambda e_, S5=S5: e_.tensor_tensor(out=ya[:], in0=S5[:, 2, :], in1=drow[:], op=ALU.mult), [e_i1, prev])
            e = P.op("dve", lambda e_, S5=S5: e_.tensor_tensor(out=ya[:], in0=ya[:], in1=S5[:, 0, :], op=ALU.add), [e])
            e = P.op("dve", lambda e_, S5=S5: e_.tensor_tensor(out=ya[:], in0=ya[:], in1=S5[:, 1, :], op=ALU.add), [e])
            e = P.op("dve", lambda e_: e_.tensor_tensor(out=yb2[:], in0=ya[:], in1=ya[:], op=ALU.mult), [e])
            e = P.op("dve", lambda e_: e_.tensor_scalar(out=yb2[:], in0=yb2[:], scalar1=0.044715, scalar2=1.0, op0=ALU.mult, op1=ALU.add), [e])
            e = P.op("dve", lambda e_: e_.tensor_tensor(out=yb2[:], in0=yb2[:], in1=ya[:], op=ALU.mult), [e])
            e = P.op("act", lambda e_: e_.activation(out=ysg[:], in_=yb2[:], func=AF.Sigmoid, scale=1.5957691216), [e])
            e = P.op("dve", lambda e_: e_.tensor_tensor(out=ya[:], in0=ya[:], in1=ysg[:], op=ALU.mult), [e])
            e_abf = P.op("act", lambda e_: e_.activation(out=a_bf[:], in_=ya[:], func=AF.Copy), [e])
            ep = None
            for k in range(4):
                ep = P.op("pe", lambda e_, k=k: e_.transpose(out=tpb_bf[:, k * 128:(k + 1) * 128], in_=a_bf[:, k * 128:(k + 1) * 128], identity=identb[:]), [e_abf])
            e = P.op("dve", lambda e_: e_.tensor_copy(out=aT[:].rearrange("p k t -> p (k t)"), in_=tpb_bf[:, 0:512]), [ep])
            for k in range(4):
                ep = P.op("pe", lambda e_, k=k: e_.matmul(banks[1][:], lhsT=aT[:, k, :], rhs=wglu[:, k, :], start=(k == 0), stop=(k == 3)), [e] if k == 0 else [])
            e = P.op("act", lambda e_: e_.activation(out=zsg[:], in_=banks[1][:], func=AF.Sigmoid), [ep])
            e_a = P.op("dve", lambda e_: e_.tensor_tensor(out=A_bf[:, 0:512], in0=ya[:], in1=zsg[:], op=ALU.mult), [e])
            e = P.op("pool", lambda e_, RT=RT: e_.tensor_tensor(out=osum[:], in0=RT[:, 0, :], in1=RT[:, 1, :], op=ALU.add), [e_i2, prev])
            e_g = P.op("act", lambda e_, RT=RT: e_.activation(out=gsil[:], in_=RT[:, 2, :], func=AF.Sigmoid), [e_i2, prev])
            e_g = P.op("pool", lambda e_, RT=RT: e_.tensor_tensor(out=gsil[:], in0=gsil[:], in1=RT[:, 2, :], op=ALU.mult), [e_g])
            for h in range(4):
                e = P.op("act", lambda e_, h=h: e_.activation(out=scr[:, h * 128:(h + 1) * 128], in_=osum[:, h * 128:(h + 1) * 128], func=AF.Square,
                                                               accum_out=ssq4[:, h:h + 1]), [e])
            e = P.op("act", lambda e_: e_.activation(out=ssq4[:], in_=ssq4[:], func=AF.Sqrt, scale=1.0 / 128.0, bias=eps[:]), [e])
            e = P.op("dve", lambda e_: e_.reciprocal(out=ssq4[:], in_=ssq4[:]), [e, e_a])
            for h in range(4):
                e = P.op("dve", lambda e_, h=h: e_.scalar_tensor_tensor(out=A_bf[:, 512 + h * 128:512 + (h + 1) * 128], in0=osum[:, h * 128:(h + 1) * 128],
                                                                        scalar=ssq4[:, h:h + 1], in1=gsil[:, h * 128:(h + 1) * 128], op0=ALU.mult, op1=ALU.mult), [e, e_g])
            in_free[b] = e
            e_A = e
        else:
            e_i = P.dma("act", at[b][:], a_d[rows, :], after=[in_free[b]])
            e_A = P.op("act", lambda e_, b=b: e_.activation(out=A_bf[:], in_=at[b][:], func=AF.Copy), [e_i, prev])
            in_free[b] = e_A
        ep = None
        for k in range(8):
            ep = P.op("pe", lambda e_, k=k: e_.transpose(out=tpb_bf[:, k * 128:(k + 1) * 128], in_=A_bf[:, k * 128:(k + 1) * 128], identity=identb[:]), [e_A])
        e = P.op("dve", lambda e_: e_.tensor_copy(out=AT[:].rearrange("p k t -> p (k t)"), in_=tpb_bf[:, 0:1024]), [ep])
        for half in range(2):
            for k in range(8):
                ep = P.op("pe", lambda e_, k=k, half=half: e_.matmul(banks[2 + half][:], lhsT=AT[:, k, :], rhs=w_bf[:, k, half * 512:(half + 1) * 512],
                                                                     start=(k == 0), stop=(k == 7)), [e] if k == 0 else [])
        for half in range(2):
            cs_ = slice(half * 512, (half + 1) * 512)
            e = P.op("dve", lambda e_, cs_=cs_, half=half, s=s: e_.tensor_tensor(out=mt[:, cs_], in0=banks[2 + half][:], in1=g1[:, s, cs_], op=ALU.mult), [ep, e_x])
        e_x1 = P.op("pool", lambda e_, b=b: e_.tensor_tensor(out=x1[b][:], in0=mt[:], in1=xt[b][:], op=ALU.add), [e, x1_free[b]])
        x_free[b] = e_x1
        e_o1 = P.dma("pool", x1_d[rows, :], x1[b][:], after=[e_x1])
        evs = emit_norm_mod_T(P, x1[b], 2, s, A_sb, B_sb, ident, [banks[4], banks[5]], hT[b], scr2, ssq, rstd, xn,
                              after_x=[e_x1], after_free=[h_free[b]])
        x1_free[b] = evs[-1]
        e_o2 = P.dma("pool", h2_d[:, :, rows].rearrange("k p t -> p k t"), hT[b][:], after=evs)
        for k in range(8):
            ep = P.op("pe", lambda e_, k=k, b=b: e_.matmul(banks[6][:, 0:32], lhsT=hT[b][:, k, :], rhs=wr[:, k, :], start=(k == 0), stop=(k == 7)), evs if k == 0 else [])
        h_free[b] = e_o2
        e = P.op("dve", lambda e_: e_.tensor_tensor(out=lgt[:], in0=banks[6][:, 0:32], in1=br[:], op=ALU.add), [ep])
        e = P.op("dve", lambda e_: e_.max(out=m8[:], in_=lgt[:]), [e])
        e = P.op("dve", lambda e_: e_.tensor_scalar(out=msk[:], in0=lgt[:], scalar1=m8[:, 3:4], scalar2=None, op0=ALU.is_ge), [e])
        e = P.op("dve", lambda e_: e_.tensor_scalar(out=nmx[:], in0=m8[:, 0:1], scalar1=-1.0, scalar2=None, op0=ALU.mult), [e])
        e = P.op("act", lambda e_: e_.activation(out=ex[:], in_=lgt[:], func=AF.Exp, bias=nmx[:]), [e])
        e = P.op("dve", lambda e_: e_.tensor_tensor(out=ex[:], in0=ex[:], in1=msk[:], op=ALU.mult), [e])
        e = P.op("dve", lambda e_: e_.tensor_reduce(out=ssum[:], in_=ex[:], axis=AX.X, op=ALU.add), [e])
        e = P.op("dve", lambda e_: e_.reciprocal(out=ssum[:], in_=ssum[:]), [e])
        e = P.op("dve", lambda e_, b=b: e_.tensor_scalar(out=gts[b][:], in0=ex[:], scalar1=ssum[:, 0:1], scalar2=None, op0=ALU.mult), [e, g_free[b]])
        e_o3 = P.dma("pool", gt_d[rows, :], gts[b][:], after=[e])
        g_free[b] = e_o3
        x1_free[b] = e_o1
        prev = e
        outs += [e_o1, e_o2, e_o3]
    P.emit(outs[-6:])
    return nc


def bc_rows(v):
    v = np.asarray(v, np.float32)
    return np.ascontiguousarray(np.broadcast_to(v[None], (128,) + v.shape))


def kt_layout(W):
    return np.ascontiguousarray(np.asarray(W, np.float32).reshape(8, 128, -1).transpose(1, 0, 2))


def run_k3(even, tset, x_tiles, ins, W, g1s, nw, sc2, sh2, w_router, b_router, extra=None):
    NT = x_tiles[0].shape[0] // 128
    nc = build_k3(NT, tset, even)
    ident = np.eye(128, dtype=np.float32)
    in_maps = []
    for c in range(NCORES):
        m = {"x": x_tiles[c], "w": kt_layout(W), "g1": bc_rows(g1s[c]), "nw": fm_cols(nw), "sc": fm_cols(sc2[c]), "sh": fm_cols(sh2[c]),
             "wr": kt_layout(w_router), "br": bc_rows(b_router), "ident": ident}
        m.update(ins[c])
        if even:
            m["drow"] = bc_rows(extra["d"]); m["wglu"] = np.ascontiguousarray(extra["wglu"].reshape(4, 128, 512).transpose(1, 0, 2))
        in_maps.append(m)
    res = _run(nc, in_maps)
    return [(res[c]["x1"], res[c]["h2T"], res[c]["gates"]) for c in range(NCORES)]


def build_k5(NT, NE=32):
    nc = bass.Bass("TRN2", target_bir_lowering=False)
    T = NT * 128
    dt = lambda n, s: nc.dram_tensor(n, s, F32, kind="ExternalInput").ap()
    h_d = dt("h2T", [8, 128, T])
    g_d = dt("gpm", [NE, 128, NT])
    w1_d = dt("w1", [NE, 1024, 2048]); w2_d = dt("w2", [NE, 1024, 1024])
    b1_d = dt("b1", [NE, 128, 16]); b2_d = dt("b2", [NE, 1, 1024])
    yz_d = dt("yzero", [T, 1024])
    y_d = nc.dram_tensor("y", [T, 1024], F32, kind="ExternalOutput").ap()
    P = Prog(nc)
    groups = [(s, min(512, T - s)) for s in range(0, T, 512)]
    w1b = P.sb([128, 8, 2048], BF16); w2b = P.sb([128, 8, 1024], BF16)
    b1s = P.sb([128, 16]); b2s = P.sb([1, 1024], BF16); ones = P.sb([1, 128], BF16); gs = P.sb([128, NT])
    hb = [P.sb([128, 8, 512], BF16) for _ in range(2)]
    act_ = [P.sb([128, 8, 512], BF16) for _ in range(2)]
    xg = [P.sb([128, 512]) for _ in range(2)]; sg = [P.sb([128, 512]) for _ in range(2)]; xl = [P.sb([128, 512]) for _ in range(2)]
    ys = [P.sb([128, 1024]) for _ in range(2)]
    banks = [P.ps([128, 512]) for _ in range(8)]
    PG = banks[0:2]; PL = banks[2:4]; PY = banks[4:8]
    e_on = P.op("pool", lambda e: e.memset(ones[:], 1.0))
    e_y0 = P.dma("sp", y_d, yz_d)
    P.loop_begin(NE)
    L = lambda f: f
    ew = []
    for kt in range(8):
        ew.append(P.dma("pool", None, None, ch="w1", fn=lambda e, i, kt=kt: e.dma_start(out=w1b[:, kt, :], in_=w1_d[i, kt * 128:(kt + 1) * 128, :])))
    for kt in range(8):
        ew.append(P.dma("pool", None, None, ch="w2", fn=lambda e, i, kt=kt: e.dma_start(out=w2b[:, kt, :], in_=w2_d[i, kt * 128:(kt + 1) * 128, :])))
    ew.append(P.dma("sp", None, None, ch="b1", fn=lambda e, i: e.dma_start(out=b1s[:], in_=b1_d[i])))
    ew.append(P.dma("pool", None, None, ch="b2", fn=lambda e, i: e.dma_start(out=b2s[:], in_=b2_d[i])))
    ew.append(P.dma("sp", None, None, ch="gs", fn=lambda e, i: e.dma_start(out=gs[:], in_=g_d[i])))
    hb_free = [None, None]; act_free = [None, None]; x_free = [None, None]; ys_free = [None, None]
    pg_free = [None, None]; py_free = [None] * 4
    ui = 0; pyi = 0; ysi = 0
    for gi, (s0, n) in enumerate(groups):
        hbuf = gi % 2
        e_h = P.dma("pool", None, None, after=[hb_free[hbuf]], ch=f"h{hbuf}",
                    fn=lambda e, i, s0=s0, n=n, hbuf=hbuf: e.dma_start(out=hb[hbuf][:, :, 0:n], in_=h_d[:, :, s0:s0 + n].rearrange("k p t -> p k t")))
        last_mm = None
        acts = []
        for fp in range(8):
            pb = ui % 2; ui += 1
            for kt in range(8):
                mm = P.op("pe", lambda e, i, kt=kt, fp=fp, pb=pb, hbuf=hbuf, n=n: e.matmul(PG[pb][:, 0:n], lhsT=w1b[:, kt, fp * 128:(fp + 1) * 128], rhs=hb[hbuf][:, kt, 0:n],
                                                                                         start=(kt == 0), stop=(kt == 7)),
                          ([e_h, pg_free[pb]] + ew[:16]) if kt == 0 else [])
            for kt in range(8):
                mm2 = P.op("pe", lambda e, i, kt=kt, fp=fp, pb=pb, hbuf=hbuf, n=n: e.matmul(PL[pb][:, 0:n], lhsT=w1b[:, kt, 1024 + fp * 128:1024 + (fp + 1) * 128],
                                                                                          rhs=hb[hbuf][:, kt, 0:n], start=(kt == 0), stop=(kt == 7)), [])
            last_mm = mm2
            a1 = P.op("dve", lambda e, i, fp=fp, pb=pb, n=n: e.tensor_scalar(out=xg[pb][:, 0:n], in0=PG[pb][:, 0:n], scalar1=b1s[:, fp:fp + 1], scalar2=7.0,
                                                                           op0=ALU.add, op1=ALU.min), [mm, ew[16], x_free[pb]])
            a2 = P.op("act", lambda e, i, pb=pb, n=n: e.activation(out=sg[pb][:, 0:n], in_=xg[pb][:, 0:n], func=AF.Sigmoid, scale=1.702), [a1])
            a3 = P.op("dve", lambda e, i, fp=fp, pb=pb, n=n: e.tensor_scalar(out=xl[pb][:, 0:n], in0=PL[pb][:, 0:n], scalar1=b1s[:, 8 + fp:9 + fp], scalar2=7.0,
                                                                           op0=ALU.add, op1=ALU.min), [mm2])
            pg_free[pb] = a3
            a4 = P.op("dve", lambda e, i, pb=pb, n=n: e.tensor_scalar(out=xl[pb][:, 0:n], in0=xl[pb][:, 0:n], scalar1=-7.0, scalar2=1.0, op0=ALU.max, op1=ALU.add), [a3])
            a5 = P.op("pool", lambda e, i, pb=pb, n=n: e.tensor_tensor(out=xg[pb][:, 0:n], in0=xg[pb][:, 0:n], in1=sg[pb][:, 0:n], op=ALU.mult), [a2])
            a6 = P.op("pool", lambda e, i, fp=fp, pb=pb, n=n, hbuf=hbuf: e.tensor_tensor(out=act_[hbuf][:, fp, 0:n], in0=xg[pb][:, 0:n], in1=xl[pb][:, 0:n], op=ALU.mult),
                      [a5, a4, act_free[hbuf]])
            x_free[pb] = a6
            acts.append(a6)
        hb_free[hbuf] = last_mm
        last_y = None
        for tt in range(n // 128):
            tile = (s0 // 128) + tt
            yb = ysi % 2; ysi += 1
            cps = []
            for half in range(2):
                pb = pyi % 4; pyi += 1
                for fp in range(8):
                    my = P.op("pe", lambda e, i, fp=fp, half=half, pb=pb, tt=tt, hbuf=hbuf: e.matmul(PY[pb][:], lhsT=act_[hbuf][:, fp, tt * 128:(tt + 1) * 128],
                                                                                                    rhs=w2b[:, fp, half * 512:(half + 1) * 512], start=(fp == 0), stop=False),
                              (acts + [py_free[pb]] + ew[8:16]) if fp == 0 else [])
                my = P.op("pe", lambda e, i, half=half, pb=pb: e.matmul(PY[pb][:], lhsT=ones[:], rhs=b2s[:, half * 512:(half + 1) * 512], start=False, stop=True), [ew[17], e_on])
                last_y = my
                cp = P.op("act" if half == 0 else "dve",
                          (lambda e, i, half=half, pb=pb, yb=yb, tile=tile: e.activation(out=ys[yb][:, half * 512:(half + 1) * 512], in_=PY[pb][:], func=AF.Copy, scale=gs[:, tile:tile + 1]))
                          if half == 0 else
                          (lambda e, i, half=half, pb=pb, yb=yb, tile=tile: e.tensor_scalar(out=ys[yb][:, half * 512:(half + 1) * 512], in0=PY[pb][:], scalar1=gs[:, tile:tile + 1],
                                                                                           scalar2=None, op0=ALU.mult)),
                          [my, ew[18], ys_free[yb]])
                py_free[pb] = cp
                cps.append(cp)
            e_acc = P.dma("pool", None, None, after=cps + [e_y0], ch=f"ya{yb}",
                          fn=lambda e, i, tile=tile, yb=yb: e.dma_start(out=y_d[tile * 128:(tile + 1) * 128, :], in_=ys[yb][:], accum_op=ALU.add))
            ys_free[yb] = e_acc
        act_free[hbuf] = last_y
    P.loop_end()
    P.emit([ys_free[0], ys_free[1]])
    return nc


def run_k5(h2T, gates, w1, b1, w2, b2, NE=32):
    T = h2T[0].shape[2]
    NT = T // 128
    nc = build_k5(NT, NE)
    b1l = np.ascontiguousarray(b1.reshape(NE, 16, 128).transpose(0, 2, 1))
    b2l = np.ascontiguousarray(b2.reshape(NE, 1, 1024))
    yz = np.zeros((T, 1024), np.float32)
    in_maps = []
    for c in range(NCORES):
        gpm = np.ascontiguousarray(gates[c].reshape(NT, 128, NE).transpose(2, 1, 0))
        in_maps.append({"h2T": h2T[c], "gpm": gpm, "w1": w1, "w2": w2, "b1": b1l, "b2": b2l, "yzero": yz})
    res = _run(nc, in_maps)
    return [res[c]["y"] for c in range(NCORES)]


def build_k4(NL=28, NS=4, dbg=0):
    NP = NL + NS
    nc = bass.Bass("TRN2", target_bir_lowering=False)
    dt = lambda n, s: nc.dram_tensor(n, s, F32, kind="ExternalInput").ap()
    q_d = dt("qp", [NP, 128, 8, 128]); k_d = dt("kp", [NP, 128, 8, 576]); v_d = dt("vp", [NP, 128, 5, 1024])
    kc_d = dt("kc", [128, 8, 256]); vc_d = dt("vc", [128, 2, 1024])
    b_d = dt("bias", [5, 128, 16, 576]); id_d = dt("ident", [128, 128])
    o_d = nc.dram_tensor("o", [NP, 128, 1024], F32, kind="ExternalOutput").ap()
    P = Prog(nc)
    ident = P.sb([128, 128]); identb = P.sb([128, 128], BF16)
    Kc = P.sb([128, 8, 256], BF16); Vc = P.sb([128, 2, 1024], BF16)
    tb0 = P.sb([128, 16, 576], BF16); tbx = P.sb([128, 16, 576], BF16)
    Qb = P.sb([128, 8, 128], BF16); Kb = P.sb([128, 8, 576], BF16); Vb = P.sb([128, 5, 1024], BF16)
    sc = [P.sb([128, 832]) for _ in range(2)]; pb_ = [P.sb([128, 832], BF16) for _ in range(2)]
    PT = [P.sb([128, 7, 128], BF16) for _ in range(2)]
    mx = [P.sb([128, 1]) for _ in range(2)]; ssum = [P.sb([128, 1]) for _ in range(2)]
    O = P.sb([128, 1024])
    banks = [P.ps([128, 512]) for _ in range(8)]
    c0 = [P.dma("sp", ident[:], id_d), P.dma("pool", Kc[:], kc_d), P.dma("pool", Vc[:], vc_d), P.dma("pool", tb0[:], b_d[0])]
    e_id = P.op("pool", lambda e: e.tensor_copy(out=identb[:], in_=ident[:]), c0)

    def pair(idx_of, tbl, pre, tag):
        L = (lambda f: (lambda e, i=None: f(e, i)))
        eq = P.dma("pool", None, None, after=pre, ch="q" + tag, fn=L(lambda e, i: e.dma_start(out=Qb[:], in_=q_d[idx_of(i)])))
        ek = P.dma("pool", None, None, after=pre, ch="k" + tag, fn=L(lambda e, i: e.dma_start(out=Kb[:], in_=k_d[idx_of(i)])))
        ev = P.dma("pool", None, None, after=pre, ch="v" + tag, fn=L(lambda e, i: e.dma_start(out=Vb[:], in_=v_d[idx_of(i)])))
        frees = {}
        last = None
        o_evs = []
        for h in range(16):
            hp, par = h // 2, h % 2
            ps_ = slice(64 * par, 64 * par + 64)
            hb = h % 2
            A, B, C, D = banks[hb * 4], banks[hb * 4 + 1], banks[hb * 4 + 2], banks[hb * 4 + 3]
            Cb = C[:].bitcast(BF16)
            fr = frees.get(hb, [])
            m1 = P.op("pe", L(lambda e, i, hp=hp, ps_=ps_, A=A: e.matmul(A[:], lhsT=Qb[ps_, hp, :], rhs=Kb[ps_, hp, 0:512], start=True, stop=True)), [eq, ek, e_id] + fr)
            m2 = P.op("pe", L(lambda e, i, hp=hp, ps_=ps_, B=B: e.matmul(B[:, 0:64], lhsT=Qb[ps_, hp, :], rhs=Kb[ps_, hp, 512:576], start=True, stop=True)), [])
            m3 = P.op("pe", L(lambda e, i, hp=hp, ps_=ps_, B=B: e.matmul(B[:, 64:320], lhsT=Qb[ps_, hp, :], rhs=Kc[ps_, hp, :], start=True, stop=True)), [])
            d1 = P.op("dve", L(lambda e, i, h=h, hb=hb, A=A: e.scalar_tensor_tensor(out=sc[hb][:, 0:512], in0=A[:], scalar=0.125, in1=tbl[:, h, 0:512], op0=ALU.mult, op1=ALU.add)), [m1] + fr)
            d2 = P.op("dve", L(lambda e, i, h=h, hb=hb, B=B: e.scalar_tensor_tensor(out=sc[hb][:, 512:576], in0=B[:, 0:64], scalar=0.125, in1=tbl[:, h, 512:576], op0=ALU.mult, op1=ALU.add)), [m3])
            d3 = P.op("dve", L(lambda e, i, hb=hb, B=B: e.tensor_scalar(out=sc[hb][:, 576:832], in0=B[:, 64:320], scalar1=0.125, scalar2=None, op0=ALU.mult)), [d2])
            d4 = P.op("dve", L(lambda e, i, hb=hb: e.tensor_reduce(out=mx[hb][:], in_=sc[hb][:], axis=AX.X, op=ALU.max)), [d1, d3])
            d5 = P.op("dve", L(lambda e, i, hb=hb: e.tensor_scalar(out=mx[hb][:], in0=mx[hb][:], scalar1=-1.0, scalar2=None, op0=ALU.mult)), [d4])
            a1 = P.op("act", L(lambda e, i, hb=hb: e.activation(out=pb_[hb][:], in_=sc[hb][:], func=AF.Exp, bias=mx[hb][:], accum_out=ssum[hb][:])), [d5])
            d6 = P.op("dve", L(lambda e, i, hb=hb: e.reciprocal(out=ssum[hb][:], in_=ssum[hb][:])), [a1])
            if dbg == 1:
                oe = P.op("dve", L(lambda e, i, hb=hb, h=h: e.tensor_copy(out=O[:, h * 64:(h + 1) * 64], in_=sc[hb][:, 0:64])), [d6])
                frees[hb] = [oe]; o_evs.append(oe)
                continue
            tp = None
            for c in range(7):
                lo = c * 128 if c < 5 else 576 + (c - 5) * 128
                w = 64 if c == 4 else 128
                tp = P.op("pe", L(lambda e, i, hb=hb, c=c, lo=lo, w=w, Cb=Cb: e.transpose(out=Cb[0:w, c * 128:(c + 1) * 128], in_=pb_[hb][:, lo:lo + w], identity=identb[:])), [a1] if c == 0 else [])
            a2 = P.op("act", L(lambda e, i, hb=hb, Cb=Cb: e.activation(out=PT[hb][:].rearrange("p c t -> p (c t)"), in_=Cb[:, 0:896], func=AF.Copy)), [tp])
            if dbg == 2:
                oe = P.op("dve", L(lambda e, i, hb=hb, h=h: e.tensor_copy(out=O[:, h * 64:(h + 1) * 64], in_=PT[hb][:, 0, 0:64])), [a2, d6])
                frees[hb] = [oe]; o_evs.append(oe)
                continue
            pv = None
            for c in range(7):
                if c < 4:
                    f = L(lambda e, i, hb=hb, c=c, h=h, D=D: e.matmul(D[:, 0:64], lhsT=PT[hb][:, c, :], rhs=Vb[:, c, h * 64:(h + 1) * 64], start=(c == 0), stop=False))
                elif c == 4:
                    f = L(lambda e, i, hb=hb, c=c, h=h, D=D: e.matmul(D[:, 0:64], lhsT=PT[hb][0:64, 4, :], rhs=Vb[0:64, 4, h * 64:(h + 1) * 64], start=False, stop=False))
                else:
                    f = L(lambda e, i, hb=hb, c=c, h=h, D=D: e.matmul(D[:, 0:64], lhsT=PT[hb][:, c, :], rhs=Vc[:, c - 5, h * 64:(h + 1) * 64], start=False, stop=(c == 6)))
                pv = P.op("pe", f, [a2, ev] if c == 0 else [])
            oe = P.op("dve", L(lambda e, i, hb=hb, h=h, D=D: e.tensor_scalar(out=O[:, h * 64:(h + 1) * 64], in0=D[:, 0:64], scalar1=ssum[hb][:, 0:1], scalar2=None, op0=ALU.mult)), [pv, d6])
            frees[hb] = [oe]
            o_evs.append(oe)
        e_out = P.dma("pool", None, None, after=o_evs, ch="o" + tag, fn=L(lambda e, i: e.dma_start(out=o_d[idx_of(i)], in_=O[:])))
        return e_out

    P.loop_begin(NL)
    pair(lambda i: i, tb0, [], "L")
    P.loop_end()
    prev = []
    last = None
    for s in range(NS):
        e_t = P.dma("pool", tbx[:], b_d[1 + s], after=prev)
        last = pair(lambda i, s=s: NL + s, tbx, [e_t], "S")
        prev = [last]
    P.emit([last])
    return nc


def _na_bias_table(rpb, r):
    bs = min(max(r - 4, 0), 248)
    tbl = np.full((128, 16, 9, 64), -30000.0, np.float32)
    col = np.arange(64)
    cs = np.clip(col - 8, 0, 48)
    for qi in range(2):
        rq = r + qi
        rs = min(max(rq - 4, 0), 248)
        for j in range(9):
            kr = bs + j
            if kr < rs or kr >= rs + 8 or kr > 255:
                continue
            for c in range(64):
                kc = np.arange(cs[c], cs[c] + 16)
                tbl[qi * 64 + c, :, j, kc] = rpb[:, kr - rq + 7, kc - c + 15].T
    return tbl.reshape(128, 16, 576)


def run_k4(qkv_lat, qkv_ctx, rpb):
    nc = build_k4()
    ident = np.eye(128, dtype=np.float32)
    order = list(range(2, 30)) + [0, 1, 30, 31]
    in_maps = []
    for core in range(NCORES):
        b, s = core // 4, core % 4
        r0 = 64 * s
        grid = qkv_lat[b].reshape(256, 64, 3, 16, 64)
        qp = np.zeros((32, 128, 8, 128), np.float32); kp = np.zeros((32, 128, 8, 576), np.float32); vp = np.zeros((32, 128, 5, 1024), np.float32)
        for n, p in enumerate(order):
            r = r0 + 2 * p
            q = grid[r:r + 2, :, 0].reshape(128, 8, 2, 64)
            qp[n] = q.transpose(2, 3, 1, 0).reshape(128, 8, 128)
            bs = min(max(r - 4, 0), 248)
            nr = min(9, 256 - bs)
            kb = np.zeros((9, 64, 16, 64), np.float32); vb = np.zeros((9, 64, 16, 64), np.float32)
            kb[:nr] = grid[bs:bs + nr, :, 1]; vb[:nr] = grid[bs:bs + nr, :, 2]
            kp[n] = kb.reshape(576, 8, 2, 64).transpose(2, 3, 1, 0).reshape(128, 8, 576)
            vv = np.zeros((640, 1024), np.float32); vv[:576] = vb.reshape(576, 1024)
            vp[n] = vv.reshape(5, 128, 1024).transpose(1, 0, 2)
        cq = qkv_ctx[b].reshape(256, 3, 16, 64)
        kc = cq[:, 1].reshape(256, 8, 2, 64).transpose(2, 3, 1, 0).reshape(128, 8, 256)
        vc = cq[:, 2].reshape(2, 128, 1024).transpose(1, 0, 2)
        bias = np.stack([_na_bias_table(rpb, 100)] + [_na_bias_table(rpb, r0 + 2 * p) for p in (0, 1, 30, 31)])
        in_maps.append({"qp": qp, "kp": kp, "vp": vp, "kc": np.ascontiguousarray(kc), "vc": np.ascontiguousarray(vc), "bias": bias, "ident": ident})
    res = _run(nc, in_maps)
    out = np.zeros((2, 16384, 1024), np.float32)
    for core in range(NCORES):
        b, s = core // 4, core % 4
        o = res[core]["o"]
        for n, p in enumerate(order):
            t0 = (64 * s + 2 * p) * 64
            out[b, t0:t0 + 128] = o[n]
    return out


NTOK = 33
_DBG = {}


def _core_rows(lat, ctx):
    F_ = lat.shape[-1]
    out = []
    for c in range(NCORES):
        b, s = c // 4, c % 4
        a = np.zeros((NTOK * 128, F_), np.float32)
        a[:4096] = lat[b, s * 4096:(s + 1) * 4096]
        if ctx is not None:
            a[4096:4160] = ctx[b, s * 64:(s + 1) * 64]
        out.append(a)
    return out


def _uncore(tiles, F_):
    lat = np.zeros((2, 16384, F_), np.float32); ctx = np.zeros((2, 256, F_), np.float32)
    for c in range(NCORES):
        b, s = c // 4, c % 4
        lat[b, s * 4096:(s + 1) * 4096] = tiles[c][:4096]
        ctx[b, s * 64:(s + 1) * 64] = tiles[c][4096:4160]
    return lat, ctx


def _sets(mods, l, k):
    return [np.stack([mods[l, c // 4, k * 1024:(k + 1) * 1024], mods[l, 2, k * 1024:(k + 1) * 1024]]) for c in range(NCORES)]


def kernel(x, c, ctx, c_ctx, ada_w, ada_b, norm_w, final_norm_w, ev_w_in, ev_w_out,
           s5_lam_re, s5_lam_im, s5_log_step, s5_b_re, s5_b_im, s5_c_re, s5_c_im, s5_d, s5_w_glu,
           ret_log_decay, na_w_qkv, na_w_o, na_rpb,
           moe_w_router, moe_b_router, moe_w1, moe_b1, moe_w2, moe_b2):
    A = lambda a: np.asarray(a, np.float32)
    x, c, ctx, c_ctx = A(x), A(c), A(ctx), A(c_ctx)
    tset = [0] * 32 + [1]
    mods = run_k0(c, c_ctx, A(ada_w), A(ada_b))
    _DBG["mods"] = mods
    xt = _core_rows(x, ctx)
    p = run_k1(xt, tset, A(norm_w)[0, 0], _sets(mods, 0, 1), _sets(mods, 0, 0), A(ev_w_in)[0])
    p_lat, p_ctx = _uncore(p, 2560)
    _DBG["p_lat"] = p_lat
    prm = {"s5_lam_re": A(s5_lam_re), "s5_lam_im": A(s5_lam_im), "s5_log_step": A(s5_log_step), "s5_b_re": A(s5_b_re), "s5_b_im": A(s5_b_im),
           "s5_c_re": A(s5_c_re), "s5_c_im": A(s5_c_im), "ret_log_decay": A(ret_log_decay)}
    yf, of = run_k2(p_lat, p_ctx, prm, 0)
    yb, ob = run_k2(p_lat, p_ctx, prm, 1)
    unflip = lambda a: (a[:, 256:][:, ::-1], a[:, :256][:, ::-1])
    yb_lat, yb_ctx = unflip(yb); ob_lat, ob_ctx = unflip(ob)
    s5in = _core_rows(np.stack([yf[:, 256:], yb_lat, p_lat[..., 0:512]], 2).reshape(2, 16384, 1536),
                      np.stack([yf[:, :256], yb_ctx, p_ctx[..., 0:512]], 2).reshape(2, 256, 1536))
    retin = _core_rows(np.stack([of[:, 256:], ob_lat, p_lat[..., 2048:2560]], 2).reshape(2, 16384, 1536),
                       np.stack([of[:, :256], ob_ctx, p_ctx[..., 2048:2560]], 2).reshape(2, 256, 1536))
    ins = [{"s5in": s5in[i].reshape(-1, 3, 512), "retin": retin[i].reshape(-1, 3, 512)} for i in range(NCORES)]
    r3 = run_k3(True, tset, xt, ins, A(ev_w_out)[0], _sets(mods, 0, 2), A(norm_w)[0, 1], _sets(mods, 0, 4), _sets(mods, 0, 3),
                A(moe_w_router)[0], A(moe_b_router)[0], extra={"d": A(s5_d)[0], "wglu": A(s5_w_glu)[0]})
    x1 = [r[0] for r in r3]
    _DBG["x1"] = x1
    y0 = run_k5([r[1] for r in r3], [r[2] for r in r3], A(moe_w1)[0], A(moe_b1)[0], A(moe_w2)[0], A(moe_b2)[0])
    _DBG["y0"] = y0
    p1, x2 = run_k1(x1, tset, A(norm_w)[1, 0], _sets(mods, 1, 1), _sets(mods, 1, 0), A(na_w_qkv)[0], y_tiles=y0, g2s=_sets(mods, 0, 5))
    _DBG["x2"] = x2
    qkv_lat, qkv_ctx = _uncore(p1, 3072)
    o_att = run_k4(qkv_lat, qkv_ctx, A(na_rpb)[0])
    _DBG["o_att"] = o_att
    ins = [{"a": a} for a in _core_rows(o_att, None)]
    r3b = run_k3(False, tset, x2, ins, A(na_w_o)[0], _sets(mods, 1, 2), A(norm_w)[1, 1], _sets(mods, 1, 4), _sets(mods, 1, 3),
                 A(moe_w_router)[1], A(moe_b_router)[1])
    x3 = [r[0] for r in r3b]
    _DBG["x3"] = x3
    y1 = run_k5([r[1] for r in r3b], [r[2] for r in r3b], A(moe_w1)[1], A(moe_b1)[1], A(moe_w2)[1], A(moe_b2)[1])
    outs = run_k6(x3, y1, [mods[1, cc // 4, 5 * 1024:6 * 1024] for cc in range(NCORES)], A(final_norm_w))
    out, _ = _uncore(outs, 1024)
    return out
```
